# Optimizing a Trainium2 kernel written in Bass

```python
import jax, jax.numpy as jnp
from jax import lax
import numpy as np

D_MODEL = 2048
BATCH = 4
SEQ = 2048
DEPTH = 1

CHUNK = 64
PLE_DIM = 256
LRU_WIDTH = D_MODEL // 2
LRU_HEADS = 16
LRU_HEAD_DIM = LRU_WIDTH // LRU_HEADS
CONV_WIDTH = 4
LRU_C = 8.0
POOL_WIDTH = D_MODEL // 2
POOL_WINDOWS = (2, 4, 8, 16)
POOL_GROUPS = len(POOL_WINDOWS)
POOL_GROUP_DIM = POOL_WIDTH // POOL_GROUPS
N_BRANCHES = 2
IN_COLS = 2 * LRU_WIDTH + POOL_WIDTH + N_BRANCHES * D_MODEL
N_GROUPS = 4
EXPERTS_PER_GROUP = 4
N_EXPERTS = N_GROUPS * EXPERTS_PER_GROUP
TOP_K = 2
D_EXPERT = D_MODEL // 4
EPS = 1e-6

kernel_name = "hybrid_rglru_pool_hmoe_block"


def _rms_norm(x, g):
    xf = x.astype(jnp.float32)
    y = xf * lax.rsqrt(jnp.mean(xf * xf, axis=-1, keepdims=True) + EPS) * g.astype(jnp.float32)
    return y.astype(x.dtype)


def _combine(c1, c2):
    a1, b1 = c1
    a2, b2 = c2
    return a1 * a2, a2 * b1 + b2


def _chunked_linear_scan(a, b):
    bsz, s, c = a.shape
    nc = s // CHUNK
    a_c = a.reshape(bsz, nc, CHUNK, c)
    b_c = b.reshape(bsz, nc, CHUNK, c)
    a_cum, h_loc = lax.associative_scan(_combine, (a_c, b_c), axis=2)

    def step(h_prev, inp):
        acum, hloc = inp
        hs = hloc + acum * h_prev[:, None, :]
        return hs[:, -1], hs

    h0 = jnp.zeros((bsz, c), jnp.float32)
    _, hs = lax.scan(step, h0, (a_cum.transpose(1, 0, 2, 3), h_loc.transpose(1, 0, 2, 3)))
    return hs.transpose(1, 0, 2, 3).reshape(bsz, s, c)


def _rglru_branch(x_rnn, g_rnn, conv_w, conv_b, w_rg_a, b_rg_a, w_rg_x, b_rg_x, lru_lambda):
    dt = x_rnn.dtype
    bsz, s, c = x_rnn.shape
    xc = lax.conv_general_dilated(x_rnn, conv_w, window_strides=(1,), padding=[(CONV_WIDTH - 1, 0)],
                                  dimension_numbers=("NWC", "WIO", "NWC"),
                                  feature_group_count=c) + conv_b
    xh = xc.reshape(bsz, s, LRU_HEADS, LRU_HEAD_DIM)
    r = jax.nn.sigmoid((jnp.einsum("bshi,hij->bshj", xh, w_rg_a) + b_rg_a).astype(jnp.float32))
    ig = jax.nn.sigmoid((jnp.einsum("bshi,hij->bshj", xh, w_rg_x) + b_rg_x).astype(jnp.float32))
    r = r.reshape(bsz, s, c)
    ig = ig.reshape(bsz, s, c)
    log_a = -LRU_C * r * jax.nn.softplus(-lru_lambda.astype(jnp.float32))
    a = jnp.exp(log_a)
    mult = jnp.sqrt(-jnp.expm1(2.0 * log_a))
    h = _chunked_linear_scan(a, mult * ig * xc.astype(jnp.float32))
    return (h * jax.nn.gelu(g_rnn.astype(jnp.float32))).astype(dt)


def _pool_branch(x_pool, w_pool, pool_scale):
    dt = x_pool.dtype
    bsz, s, c = x_pool.shape
    xf = x_pool.astype(jnp.float32)
    cs = jnp.concatenate([jnp.zeros((bsz, 1, c), jnp.float32), jnp.cumsum(xf, axis=1)], axis=1)
    t = jnp.arange(s)
    outs = []
    for g, w in enumerate(POOL_WINDOWS):
        sl = slice(g * POOL_GROUP_DIM, (g + 1) * POOL_GROUP_DIM)
        lo = jnp.maximum(t + 1 - w, 0)
        win_sum = cs[:, 1:, sl] - cs[:, lo, sl]
        cnt = (t + 1 - lo).astype(jnp.float32)[None, :, None]
        outs.append(win_sum / cnt - xf[..., sl])
    d = jnp.stack(outs, axis=2).astype(dt)
    y = jnp.einsum("bsgc,gcd->bsgd", d, w_pool).reshape(bsz, s, c)
    return y * pool_scale


def _mixer(x, norm1_g, w_in, conv_w, conv_b, w_rg_a, b_rg_a, w_rg_x, b_rg_x, lru_lambda,
           w_pool, pool_scale, w_branch_a, w_branch_b, w_out):
    h = _rms_norm(x, norm1_g)
    z = h @ w_in
    o1 = LRU_WIDTH
    o2 = 2 * LRU_WIDTH
    o3 = o2 + POOL_WIDTH
    o4 = o3 + D_MODEL
    x_rnn, g_rnn, x_pool = z[..., :o1], z[..., o1:o2], z[..., o2:o3]
    gate_a, gate_b = z[..., o3:o4], z[..., o4:]
    y_a = _rglru_branch(x_rnn, g_rnn, conv_w, conv_b, w_rg_a, b_rg_a, w_rg_x, b_rg_x, lru_lambda)
    y_b = _pool_branch(x_pool, w_pool, pool_scale)
    u = jax.nn.sigmoid(gate_a) * (y_a @ w_branch_a) + jax.nn.sigmoid(gate_b) * (y_b @ w_branch_b)
    return u @ w_out


def _hier_moe(x, norm2_g, w_router_group, b_router_group, w_router_expert, b_router_expert,
              w_e_gate, w_e_up, w_e_down):
    dt = x.dtype
    bsz, s, d = x.shape
    ht = _rms_norm(x, norm2_g).reshape(bsz * s, d)
    lg = (ht @ w_router_group + b_router_group).astype(jnp.float32)
    pg = jax.nn.softmax(lg, axis=-1)
    g_idx = jnp.argmax(lg, axis=-1)
    g_w = jnp.take_along_axis(pg, g_idx[:, None], axis=1)[:, 0]
    le = (ht @ w_router_expert + b_router_expert).astype(jnp.float32)
    le = le.reshape(-1, N_GROUPS, EXPERTS_PER_GROUP)
    le_sel = jnp.take_along_axis(le, g_idx[:, None, None], axis=1)[:, 0]
    pe = jax.nn.softmax(le_sel, axis=-1)
    top_v, top_i = lax.top_k(pe, TOP_K)
    top_v = top_v / jnp.sum(top_v, axis=-1, keepdims=True)
    e_idx = g_idx[:, None] * EXPERTS_PER_GROUP + top_i
    wts = g_w[:, None] * top_v
    comb = jnp.sum(jax.nn.one_hot(e_idx, N_EXPERTS, dtype=jnp.float32) * wts[..., None], axis=1)
    hg = jnp.einsum("td,edf->tef", ht, w_e_gate)
    hu = jnp.einsum("td,edf->tef", ht, w_e_up)
    act = jax.nn.silu(hg) * hu * comb[:, :, None].astype(dt)
    y = jnp.einsum("tef,efd->td", act, w_e_down)
    return y.reshape(bsz, s, d)


def _per_layer_embed(x, p_i, norm_ple_g, w_ple_gate, w_ple_proj):
    e = p_i.astype(x.dtype) @ w_ple_proj
    g = jax.nn.sigmoid(_rms_norm(x, norm_ple_g) @ w_ple_gate)
    return g * e


def setup_inputs(seed: int = 0) -> dict:
    key = jax.random.key(seed)
    ks = iter(jax.random.split(key, 40))

    def nrm(shape, scale):
        return jax.random.normal(next(ks), shape, jnp.float32) * scale

    a0 = jax.random.uniform(next(ks), (DEPTH, LRU_WIDTH), jnp.float32, minval=0.9, maxval=0.999)
    sa = a0 ** (1.0 / LRU_C)
    lru_lambda = jnp.log(sa) - jnp.log1p(-sa)
    return {
        "x": nrm((BATCH, SEQ, D_MODEL), 1.0),
        "p": nrm((DEPTH, BATCH, SEQ, PLE_DIM), 1.0),
        "norm1_g": 1.0 + nrm((DEPTH, D_MODEL), 0.02),
        "w_in": nrm((DEPTH, D_MODEL, IN_COLS), D_MODEL ** -0.5),
        "conv_w": nrm((DEPTH, CONV_WIDTH, 1, LRU_WIDTH), CONV_WIDTH ** -0.5),
        "conv_b": nrm((DEPTH, LRU_WIDTH), 0.01),
        "w_rg_a": nrm((DEPTH, LRU_HEADS, LRU_HEAD_DIM, LRU_HEAD_DIM), LRU_HEAD_DIM ** -0.5),
        "b_rg_a": nrm((DEPTH, LRU_HEADS, LRU_HEAD_DIM), 0.01),
        "w_rg_x": nrm((DEPTH, LRU_HEADS, LRU_HEAD_DIM, LRU_HEAD_DIM), LRU_HEAD_DIM ** -0.5),
        "b_rg_x": nrm((DEPTH, LRU_HEADS, LRU_HEAD_DIM), 0.01),
        "lru_lambda": lru_lambda,
        "w_pool": nrm((DEPTH, POOL_GROUPS, POOL_GROUP_DIM, POOL_GROUP_DIM), POOL_GROUP_DIM ** -0.5),
        "pool_scale": 1.0 + nrm((DEPTH, POOL_WIDTH), 0.02),
        "w_branch_a": nrm((DEPTH, LRU_WIDTH, D_MODEL), LRU_WIDTH ** -0.5),
        "w_branch_b": nrm((DEPTH, POOL_WIDTH, D_MODEL), POOL_WIDTH ** -0.5),
        "w_out": nrm((DEPTH, D_MODEL, D_MODEL), D_MODEL ** -0.5),
        "norm2_g": 1.0 + nrm((DEPTH, D_MODEL), 0.02),
        "w_router_group": nrm((DEPTH, D_MODEL, N_GROUPS), D_MODEL ** -0.5),
        "b_router_group": nrm((DEPTH, N_GROUPS), 0.01),
        "w_router_expert": nrm((DEPTH, D_MODEL, N_EXPERTS), D_MODEL ** -0.5),
        "b_router_expert": nrm((DEPTH, N_EXPERTS), 0.01),
        "w_e_gate": nrm((DEPTH, N_EXPERTS, D_MODEL, D_EXPERT), D_MODEL ** -0.5),
        "w_e_up": nrm((DEPTH, N_EXPERTS, D_MODEL, D_EXPERT), D_MODEL ** -0.5),
        "w_e_down": nrm((DEPTH, N_EXPERTS, D_EXPERT, D_MODEL), D_EXPERT ** -0.5),
        "norm_ple_g": 1.0 + nrm((DEPTH, D_MODEL), 0.02),
        "w_ple_gate": nrm((DEPTH, D_MODEL, D_MODEL), D_MODEL ** -0.5),
        "w_ple_proj": nrm((DEPTH, PLE_DIM, D_MODEL), PLE_DIM ** -0.5),
        "final_norm_g": 1.0 + nrm((D_MODEL,), 0.02),
    }


def reference(x, p, norm1_g, w_in, conv_w, conv_b, w_rg_a, b_rg_a, w_rg_x, b_rg_x, lru_lambda,
              w_pool, pool_scale, w_branch_a, w_branch_b, w_out, norm2_g, w_router_group,
              b_router_group, w_router_expert, b_router_expert, w_e_gate, w_e_up, w_e_down,
              norm_ple_g, w_ple_gate, w_ple_proj, final_norm_g):
    for i in range(DEPTH):
        x = x + _mixer(x, norm1_g[i], w_in[i], conv_w[i], conv_b[i], w_rg_a[i], b_rg_a[i],
                       w_rg_x[i], b_rg_x[i], lru_lambda[i], w_pool[i], pool_scale[i],
                       w_branch_a[i], w_branch_b[i], w_out[i])
        x = x + _hier_moe(x, norm2_g[i], w_router_group[i], b_router_group[i], w_router_expert[i],
                          b_router_expert[i], w_e_gate[i], w_e_up[i], w_e_down[i])
        x = x + _per_layer_embed(x, p[i], norm_ple_g[i], w_ple_gate[i], w_ple_proj[i])
    return _rms_norm(x, final_norm_g)
```

```python
import numpy as np
import concourse.bass as bass
import concourse.mybir as mybir
from contextlib import ExitStack

F32 = mybir.dt.float32
BF16 = mybir.dt.bfloat16
I32 = mybir.dt.int32
AF = mybir.ActivationFunctionType
ALU = mybir.AluOpType
AX = mybir.AxisListType
_DSZ = {F32: 4, BF16: 2, mybir.dt.int32: 4, mybir.dt.uint32: 4}


def _region(ap):
    tname = type(ap.tensor).__name__
    if "DRam" in tname:
        return None
    key = "PS" if "PSum" in tname else ap.tensor.name
    sz = _DSZ[ap.dtype]
    dims = ap.ap
    pstep, pcnt = dims[0]
    off = int(ap.offset)
    if pstep > 0:
        p0 = off // pstep
        f0 = off % pstep
    else:
        p0, f0 = 0, off
    free = [(s, c) for (s, c) in dims[1:] if c > 1]
    if key == "PS":
        ext = 1
        for s_, c_ in free:
            ext += (c_ - 1) * abs(s_)
        lo = (f0 * sz) // 2048 * 2048
        hi = -(-((f0 + ext) * sz) // 2048) * 2048
        return key, 0, 128, [(lo, hi)]
    if not free:
        return key, p0, p0 + pcnt, [(f0 * sz, (f0 + 1) * sz)]
    ls, lc = free[-1]
    outer = free[:-1]
    n_outer = 1
    for s, c in outer:
        n_outer *= c
    run = (lc - 1) * abs(ls) + 1
    if n_outer <= 64:
        offs = [0]
        for s, c in outer:
            offs = [o + i * s for o in offs for i in range(c)]
        ivs = sorted((f0 + o) for o in offs)
        out = []
        for o in ivs:
            lo, hi = o * sz, (o + run) * sz
            if out and lo <= out[-1][1]:
                out[-1] = (out[-1][0], max(out[-1][1], hi))
            else:
                out.append((lo, hi))
        return key, p0, p0 + pcnt, out
    ext = run
    for s, c in outer:
        ext += (c - 1) * abs(s)
    return key, p0, p0 + pcnt, [(f0 * sz, (f0 + ext) * sz)]


def _iv_overlap(a, b):
    i = j = 0
    while i < len(a) and j < len(b):
        if a[i][1] <= b[j][0]:
            i += 1
        elif b[j][1] <= a[i][0]:
            j += 1
        else:
            return True
    return False


def _iv_covers(a, b):
    for lo, hi in b:
        ok = False
        for alo, ahi in a:
            if alo <= lo and hi <= ahi:
                ok = True
                break
        if not ok:
            return False
    return True


class Op:
    __slots__ = ("eng", "fn", "tl", "tpos", "waits", "need_inc", "semval", "done", "is_dma", "idx", "extra", "cond")


class Sched:
    ENGS = ("pe", "act", "dve", "pool", "sp")

    def __init__(self, nc):
        self.nc = nc
        self.ops = []
        self.by_eng = {e: [] for e in self.ENGS}
        self.eclock = {e: {} for e in self.ENGS}
        self.recs = {}
        self.tl_count = {}
        self.dma_group_total = {}
        self._cond = None
        self._cond_snap = None
        self.regs = {}
        self.dummy = {}

    def cond_begin(self, tag, regkey):
        if self._cond is None:
            self._cond = ()
            self._cond_snaps = []
        self._cond = self._cond + ((tag, regkey),)
        self._cond_snaps.append({e: dict(c) for e, c in self.eclock.items()})
        self._cond_snap = self._cond_snaps[0]

    def cond_end(self):
        self.eclock = self._cond_snaps.pop()
        self._cond = self._cond[:-1]
        if not self._cond:
            self._cond = None
            self._cond_snap = None

    def flagload(self, eng, key, flag_ap):
        def fn(e, eng=eng, key=key):
            if (eng, key) not in self.regs:
                self.regs[(eng, key)] = e.alloc_register("fl_%s_%s" % (eng, key))
            return e.reg_load(self.regs[(eng, key)], flag_ap)
        return self.add(eng, fn, reads=[flag_ap])

    def add(self, eng, fn, reads=(), writes=(), dma_key=None, extra_deps=()):
        op = Op()
        op.eng = eng
        op.fn = fn
        op.is_dma = dma_key is not None
        op.tl = ("dma", dma_key) if op.is_dma else eng
        op.tpos = self.tl_count.get(op.tl, 0) + 1
        self.tl_count[op.tl] = op.tpos
        op.need_inc = op.is_dma
        op.semval = None
        op.waits = []
        op.idx = len(self.ops)
        op.extra = None
        op.cond = self._cond if self._cond is not None else ()
        assert not (op.is_dma and op.cond)
        deps = list(extra_deps)
        accs = []
        for ap in reads:
            r = _region(ap)
            if r is not None:
                accs.append((r, False))
        for ap in writes:
            r = _region(ap)
            if r is not None:
                accs.append((r, True))
        for (key, p0, p1, ivs), is_w in accs:
            lst = self.recs.setdefault(key, [])
            for rec in lst:
                if not (rec[3] or is_w) and key != "PS":
                    continue
                if rec[1] <= p0 or p1 <= rec[0]:
                    continue
                if not _iv_overlap(rec[2], ivs):
                    continue
                d = rec[4]
                if d is op:
                    continue
                if (not op.is_dma) and (not d.is_dma) and d.eng == eng:
                    if eng == "pe":
                        continue
                    if not (rec[3] or is_w):
                        continue
                deps.append(d)
        clock = dict(self.eclock[eng])
        deps.sort(key=lambda d: -d.idx)
        for d in deps:
            if clock.get(d.tl, 0) >= d.tpos:
                continue
            op.waits.append(d)
            d.need_inc = True
            for k, v in d.done.items():
                if clock.get(k, 0) < v:
                    clock[k] = v
        self.eclock[eng] = clock
        if op.cond:
            op.done = dict(self._cond_snap[eng])
        else:
            op.done = dict(clock)
        op.done[op.tl] = op.tpos
        for (key, p0, p1, ivs), is_w in accs:
            lst = self.recs[key]
            if is_w:
                lst[:] = [r for r in lst if not (p0 <= r[0] and r[1] <= p1 and _iv_covers(ivs, r[2]))]
            else:
                lst[:] = [r for r in lst if not ((not r[3]) and r[4].tl == op.tl and r[0] == p0 and r[1] == p1 and r[2] == ivs)]
            lst.append([p0, p1, ivs, is_w, op])
        self.ops.append(op)
        self.by_eng[eng].append(op)
        return op

    def emit(self):
        nc = self.nc
        cnt = {}
        for op in self.ops:
            if op.is_dma:
                op.semval = 16 * op.tpos
            elif op.need_inc:
                cnt[op.tl] = cnt.get(op.tl, 0) + 1
                op.semval = cnt[op.tl]
        for k, v in cnt.items():
            assert v < 60000, (k, v)
        tls = sorted({op.tl for op in self.ops if op.need_inc}, key=str)
        with ExitStack() as es:
            sems = {}
            for i, tl in enumerate(tls):
                nm = "s_" + ("_".join(str(x) for x in tl) if isinstance(tl, tuple) else tl)
                sems[tl] = es.enter_context(nc.semaphore(nm))
            block = es.enter_context(nc.Block())

            def run(engname):
                def emit_op(eng, op):
                    for d in op.waits:
                        eng.wait_ge(sems[d.tl], d.semval)
                    ins = op.fn(eng)
                    if op.need_inc:
                        ins.then_inc(sems[op.tl], 16 if op.is_dma else 1)

                def emit_seq(eng, ops, depth):
                    i = 0
                    while i < len(ops):
                        op = ops[i]
                        if len(op.cond) <= depth:
                            emit_op(eng, op)
                            i += 1
                            continue
                        key = op.cond[depth]
                        j = i
                        while j < len(ops) and len(ops[j].cond) > depth and ops[j].cond[depth] == key:
                            j += 1
                        grp = ops[i:j]
                        ninc = sum(1 for o in grp if o.need_inc)
                        r = self.regs[(engname, key[1])]
                        with eng.If_ne(r, 0):
                            emit_seq(eng, grp, depth + 1)
                        if ninc > 0:
                            with eng.Else():
                                self.dummy[engname](eng).then_inc(sems[engname], ninc)
                        i = j

                def body(eng):
                    emit_seq(eng, self.by_eng[engname], 0)
                return body

            if self.by_eng["pe"]:
                block.tensor(run("pe"))
            if self.by_eng["act"]:
                block.scalar(run("act"))
            if self.by_eng["dve"]:
                block.vector(run("dve"))
            if self.by_eng["pool"]:
                block.gpsimd(run("pool"))
            if self.by_eng["sp"]:
                block.sync(run("sp"))

    def stats(self):
        out = {}
        for e in self.ENGS:
            ops = self.by_eng[e]
            out[e] = (len(ops), sum(len(o.waits) for o in ops), sum(1 for o in ops if o.need_inc))
        return out


def _isap(x):
    return hasattr(x, "ap") and hasattr(x, "tensor")


class K:
    def __init__(self, S):
        self.S = S
        self._dk = 0

    def mm(self, out, lhsT, rhs, start=True, stop=True):
        return self.S.add("pe", lambda e: e.matmul(out, lhsT, rhs, start=start, stop=stop),
                          reads=[lhsT, rhs], writes=[out])

    def tr(self, out, in_, ident):
        return self.S.add("pe", lambda e: e.transpose(out, in_, ident), reads=[in_, ident], writes=[out])

    def act(self, out, in_, func, bias=0.0, scale=1.0, accum_out=None):
        reads = [in_] + [x for x in (bias, scale) if _isap(x)]
        writes = [out] + ([accum_out] if accum_out is not None else [])
        if accum_out is not None:
            fn = lambda e: e.activation(out, in_, func, bias=bias, scale=scale, accum_out=accum_out)
        else:
            fn = lambda e: e.activation(out, in_, func, bias=bias, scale=scale)
        return self.S.add("act", fn, reads=reads, writes=writes)

    def tt(self, out, in0, in1, op, eng="dve"):
        return self.S.add(eng, lambda e: e.tensor_tensor(out, in0, in1, op), reads=[in0, in1], writes=[out])

    def ts(self, out, in0, s1, s2, op0, op1=None, eng="dve", accum_out=None):
        reads = [in0] + [x for x in (s1, s2) if _isap(x)]
        writes = [out] + ([accum_out] if accum_out is not None else [])
        if op1 is None:
            fn = lambda e: e.tensor_single_scalar(out, in0, s1, op0)
        elif accum_out is not None:
            fn = lambda e: e.tensor_scalar(out, in0, s1, s2, op0, op1, accum_out=accum_out)
        else:
            fn = lambda e: e.tensor_scalar(out, in0, s1, s2, op0, op1)
        return self.S.add(eng, fn, reads=reads, writes=writes)

    def stt(self, out, in0, scalar, in1, op0, op1, eng="dve"):
        reads = [in0, in1] + ([scalar] if _isap(scalar) else [])
        return self.S.add(eng, lambda e: e.scalar_tensor_tensor(out, in0, scalar, in1, op0, op1),
                          reads=reads, writes=[out])

    def scan(self, out, d0, d1, initial, op0=ALU.mult, op1=ALU.add):
        reads = [d0, d1] + ([initial] if _isap(initial) else [])
        return self.S.add("dve", lambda e: e.tensor_tensor_scan(out, d0, d1, initial, op0, op1),
                          reads=reads, writes=[out])

    def copy(self, out, in_, eng="dve"):
        return self.S.add(eng, lambda e: e.tensor_copy(out, in_), reads=[in_], writes=[out])

    def memset(self, out, val, eng="dve"):
        return self.S.add(eng, lambda e: e.memset(out, val), reads=[], writes=[out])

    def recip(self, out, in_):
        return self.S.add("dve", lambda e: e.reciprocal(out, in_), reads=[in_], writes=[out])

    def reduce(self, out, in_, op, axis=AX.X):
        return self.S.add("dve", lambda e: e.tensor_reduce(out, in_, axis, op), reads=[in_], writes=[out])

    def dma(self, out, in_, q="pool", key=None):
        if key is None:
            self._dk += 1
            key = "u%d" % self._dk
        return self.S.add(q, lambda e: e.dma_start(out=out, in_=in_), reads=[in_], writes=[out], dma_key=key)


from concourse.bass_utils import run_bass_kernel_spmd

KB = 1024
NT = 1024
D = 2048
KC = 16
EPS = 1e-6
A0, A1, RING, TT, CC = 0, 32 * KB, 96 * KB, 160 * KB, 196 * KB
ARENA_BYTES = 207 * KB
NSLOT = 8

W_SPECS = [
    ("norm1_g", [2048]), ("w_in", [2048, 7168]), ("conv_w", [4, 1024]), ("conv_b", [1024]),
    ("w_rg_a", [16, 64, 64]), ("b_rg_a", [1024]), ("w_rg_x", [16, 64, 64]), ("b_rg_x", [1024]),
    ("lru_lambda", [1024]), ("w_pool", [4, 256, 256]), ("pool_scale", [1024]),
    ("w_branch_a", [1024, 2048]), ("w_branch_b", [1024, 2048]), ("w_out", [2048, 2048]),
    ("norm2_g", [2048]), ("w_router_group", [2048, 4]), ("b_router_group", [4]),
    ("w_router_expert", [2048, 16]), ("b_router_expert", [16]),
    ("w_e_gate", [16, 2048, 512]), ("w_e_up", [16, 2048, 512]), ("w_e_down", [16, 512, 2048]),
    ("norm_ple_g", [2048]), ("w_ple_gate", [2048, 2048]), ("w_ple_proj", [256, 2048]),
    ("final_norm_g", [2048]),
]


class _Stop(Exception):
    pass


def pipeline(n, stages):
    for it in range(n + len(stages) - 1):
        for si in range(len(stages) - 1, -1, -1):
            t = it - si
            if 0 <= t < n:
                stages[si](t)


def build_program(debug=(), stop_after=None):
    nc = bass.Bass("TRN2", target_bir_lowering=False)
    dr = {}
    for nm, shp in [("xm", [NT, D]), ("xp", [NT, D]), ("pm", [NT, 256]), ("flag", [128, 1]), ("invcnt", [128, 64])] + W_SPECS:
        dr[nm] = nc.dram_tensor(nm, shp, F32, kind="ExternalInput").ap()
    out = nc.dram_tensor("out", [NT, D], F32, kind="ExternalOutput").ap()
    x1s = nc.dram_tensor("x1_scratch", [NT, D], F32, kind="Internal").ap()
    dbg_out = {}
    es = ExitStack()
    with es:
        A = es.enter_context(nc.sbuf_tensor("arena", [128, ARENA_BYTES // 2], BF16))
        PS = es.enter_context(nc.psum_tensor("ps", [128, 8, 512], F32))
        S = Sched(nc)
        k = K(S)

        def view(off, shape, dt=BF16):
            n = 1
            for s in shape[1:]:
                n *= s
            e0 = off // 2
            ne = n * (2 if dt in (F32, I32) else 1)
            assert off % 4 == 0 and off + ne * 2 <= ARENA_BYTES, (off, shape)
            v = A[:, e0:e0 + ne]
            if dt in (F32, I32):
                v = v.bitcast(dt)
            if len(shape) == 3:
                v = v.rearrange("p (a b) -> p a b", a=shape[1])
            elif len(shape) == 4:
                v = v.rearrange("p (a b c) -> p a b c", a=shape[1], b=shape[2])
            if shape[0] < 128:
                v = v[0:shape[0]]
            return v

        bank_ctr = [0]

        held = set()

        def nb():
            assert len(held) < 7, "all PSUM banks held"
            while True:
                b = bank_ctr[0] % 7
                bank_ctr[0] += 1
                if b not in held:
                    return b

        def psb(b):
            return PS[:, b, :].bitcast(BF16)

        slot_ctr = [0]

        ring_addrs = [RING + i * 8 * KB for i in range(NSLOT)]

        def wload(src, shape):
            s = slot_ctr[0] % len(ring_addrs)
            slot_ctr[0] += 1
            v = view(ring_addrs[s], shape, BF16)
            k.dma(v, src, q="pool", key="ring%d" % s)
            return v

        def dump(name, v):
            if name in debug:
                t = nc.dram_tensor("dbg_" + name, list(v.shape), v.dtype, kind="ExternalOutput").ap()
                dbg_out[name] = k.dma(t, v, q="sp")
            if stop_after == name:
                raise _Stop()

        stores = []
        try:
            ident_bf = view(CC + 0, [128, 128], BF16)
            ident_f = view(CC + 256, [128, 128], F32)
            chv = view(CC + 768, [128, 9, 8], F32)
            c1h = view(CC + 1056, [128, 8], F32)
            c1f = view(CC + 1088, [128, 8], F32)
            stg = view(CC + 1120, [72, 128], F32)
            wr = view(CC + 1632, [128, 16, 20], F32)
            rb = view(CC + 2912, [128, 20], F32)
            invc = view(CC + 2992, [128, 64], F32)
            flg = view(CC + 3248, [128, 1], F32)
            bh = view(CC + 3264, [128, 2, 8], F32)
            ss = view(CC + 3328, [128, 16], F32)
            tmpv = view(CC + 3392, [128, 16], F32)
            rstd = view(CC + 3456, [128, 16], F32)
            stt_ = view(CC + 3520, [128, 8], F32)
            tmp16 = view(CC + 3552, [128, 16], F32)
            lg = view(CC + 3616, [128, 8, 20], F32)
            comb = view(CC + 4256, [128, 8, 16], F32)
            rsc = [view(CC + 4768 + i * 128, [128, 8, 4], F32) for i in range(12)]
            rs1 = [view(CC + 6304 + i * 32, [128, 8], F32) for i in range(8)]
            prod = view(CC + 6560, [128, 8, 4, 4], F32)

            k.memset(ident_f, 0.0)
            S.add("pool", lambda e: e.affine_select(out=ident_f, in_=ident_f, pattern=[[-1, 128]],
                                                    compare_op=ALU.not_equal, fill=1.0, base=0, channel_multiplier=1),
                  reads=[ident_f], writes=[ident_f])
            k.copy(ident_bf, ident_f)
            vecs = [dr["conv_w"][0], dr["conv_w"][1], dr["conv_w"][2], dr["conv_w"][3], dr["conv_b"],
                    dr["b_rg_a"], dr["b_rg_x"], dr["lru_lambda"], dr["pool_scale"]]
            for vi, vsrc in enumerate(vecs):
                k.dma(stg[vi * 8:(vi + 1) * 8, :], vsrc.rearrange("(c p) -> c p", p=128), q="sp")
            b0 = nb()
            k.tr(PS[:, b0, 0:72], stg, ident_f[0:72, 0:72])
            k.copy(chv.rearrange("p v c -> p (v c)"), PS[:, b0, 0:72])
            k.dma(flg, dr["flag"], q="sp")
            k.dma(invc, dr["invcnt"], q="sp")
            k.act(c1f, chv[:, 7, :], AF.Exp, scale=-1.0)
            k.act(c1f, c1f, AF.Ln, bias=1.0)
            k.ts(c1h, c1f, -4.0, None, ALU.mult)
            k.ts(c1f, c1f, -8.0, None, ALU.mult)
            k.ts(bh.rearrange("p a b -> p (a b)"), chv[:, 5:7, :].rearrange("p a b -> p (a b)"), 0.5, None, ALU.mult)
            k.dma(wr[:, :, 0:4], dr["w_router_group"].rearrange("(kc p) n -> p kc n", p=128), q="sp")
            k.dma(wr[:, :, 4:20], dr["w_router_expert"].rearrange("(kc p) n -> p kc n", p=128), q="sp")
            k.dma(rb[:, 0:4], dr["b_router_group"].partition_broadcast(128), q="sp")
            k.dma(rb[:, 4:20], dr["b_router_expert"].partition_broadcast(128), q="sp")

            hTp = view(A0, [128, 16, NT], BF16)
            uT = hTp
            htT = hTp
            hpT = hTp
            hT = view(A1, [128, 16, NT], BF16)
            y_a = view(A1 + 32 * KB, [128, 8, NT], BF16)
            y_b = view(A1 + 48 * KB, [128, 8, NT], BF16)
            x1v = view(A1, [128, 8, D], F32)

            def rms_stats(src, col):
                return col

            xld = [view(A1 + 32 * KB + i * 8 * KB, [128, D], F32) for i in range(4)]
            gbc = view(TT + 16 * KB, [128, D], F32)
            xnbs = [view(TT + 24 * KB, [128, D], BF16), view(TT + 28 * KB, [128, D], BF16)]
            k.dma(gbc, dr["norm1_g"].partition_broadcast(128), q="sp")
            evc = [0]
            sqjunk = view(TT + 32 * KB, [128, D], BF16)
            n1banks = {}

            def n1_s0(t):
                src = dr["xp"] if t < 8 else dr["xm"]
                r0 = (t % 8) * 128
                xl = xld[t % 4]
                k.dma(xl, src[r0:r0 + 128, :], q="sp", key="xld%d" % (t % 4))
                k.act(sqjunk, xl, AF.Square, accum_out=ss[:, t:t + 1])

            def n1_s1(t):
                k.ts(tmpv[:, t:t + 1], ss[:, t:t + 1], 1.0 / D, EPS, ALU.mult, ALU.add)

            def n1_s2(t):
                k.act(tmpv[:, t:t + 1], tmpv[:, t:t + 1], AF.Sqrt)

            def n1_s3(t):
                k.recip(rstd[:, t:t + 1], tmpv[:, t:t + 1])
                k.stt(xnbs[t % 2], xld[t % 4], rstd[:, t:t + 1], gbc, ALU.mult, ALU.mult)

            def n1_s4(t):
                xnb = xnbs[t % 2]
                bl = []
                for h in range(2):
                    b = nb()
                    held.add(b)
                    bl.append(b)
                    pb = psb(b)
                    for j in range(8):
                        kc = h * 8 + j
                        k.tr(pb[:, j * 128:(j + 1) * 128], xnb[:, kc * 128:(kc + 1) * 128], ident_bf)
                n1banks[t] = bl

            def n1_s5(t):
                r0 = (t % 8) * 128
                dstT = hTp if t < 8 else hT
                for h, b in enumerate(n1banks[t]):
                    dst = dstT[:, h * 8:(h + 1) * 8, r0:r0 + 128]
                    srcp = psb(b).rearrange("p (a b) -> p a b", a=8)
                    if h == 0:
                        k.act(dst, srcp, AF.Identity)
                    else:
                        k.copy(dst, srcp)
                    held.discard(b)

            pipeline(16, [n1_s0, n1_s1, n1_s2, n1_s3, n1_s4, n1_s5])
            dump("hT", hT)
            dump("hTp", hTp)

            YB0 = A1 + 48 * KB
            Wbd = view(YB0 + 12 * KB, [128, 8, 2, 128], BF16)
            k.memset(Wbd.rearrange("p a b c -> p (a b c)"), 0.0)
            for gi, wnm in enumerate(["w_rg_a", "w_rg_x"]):
                wsrc = dr[wnm].rearrange("(c h) i j -> h i c j", h=2)
                for h in range(2):
                    k.dma(Wbd[h * 64:(h + 1) * 64, :, gi, h * 64:(h + 1) * 64], wsrc[h], q="pool")
            xr = view(TT + 0, [128, 1027], F32)
            xc2 = [view(TT + 4112 + i * 4096, [128, NT], F32) for i in range(2)]
            xcb2 = [view(TT + 12304 + i * 2048, [128, NT], BF16) for i in range(2)]
            Ab2 = [view(TT + 16400 + i * 4096, [128, NT], F32) for i in range(2)]
            Gb2 = [view(TT + 24592 + i * 4096, [128, NT], F32) for i in range(2)]
            Mb2 = [view(YB0 + i * 4096, [128, NT], F32) for i in range(2)]
            Yb = view(YB0 + 8 * KB, [128, NT], F32)
            w_in_v = dr["w_in"].rearrange("(kc p) c -> p kc c", p=128)
            wxs, wgs_ = {}, {}
            zbanks, ybanks = {}, {}

            def st_Z(c, hf):
                cc = c % 2
                if cc == 0 and hf == 0:
                    wxs[c // 2] = wload(w_in_v[:, :, c * 128:c * 128 + 256], [128, 16, 256])
                    wgs_[c // 2] = wload(w_in_v[:, :, 1024 + c * 128:1024 + c * 128 + 256], [128, 16, 256])
                wx = wxs[c // 2]
                hsrc = hTp if hf == 0 else hT
                bl = []
                for blk in range(2):
                    b = nb()
                    held.add(b)
                    bl.append(b)
                    for kc in range(KC):
                        k.mm(PS[:, b, :], wx[:, kc, cc * 128:(cc + 1) * 128], hsrc[:, kc, blk * 512:(blk + 1) * 512],
                             start=(kc == 0), stop=(kc == KC - 1))
                zbanks[(c, hf)] = bl

            def st_Ymm(c, hf):
                if hf == 0:
                    return
                cc = c % 2
                wg = wgs_[c // 2]
                bl = []
                for blk in range(2):
                    b = nb()
                    held.add(b)
                    bl.append(b)
                    for kc in range(KC):
                        k.mm(PS[:, b, :], wg[:, kc, cc * 128:(cc + 1) * 128], hT[:, kc, blk * 512:(blk + 1) * 512],
                             start=(kc == 0), stop=(kc == KC - 1))
                ybanks[(c, hf)] = bl

            def st_E(c, hf):
                par = (c * 2 + hf) % 2
                xc, xcb = xc2[par], xcb2[par]
                if hf == 0:
                    k.memset(xr[:, 0:3], 0.0)
                for blk, b in enumerate(zbanks[(c, hf)]):
                    k.act(xr[:, 3 + blk * 512:3 + (blk + 1) * 512], PS[:, b, :], AF.Identity)
                    held.discard(b)
                k.act(xc, xr[:, 3:3 + NT], AF.Identity, bias=chv[:, 4, c:c + 1], scale=chv[:, 3, c:c + 1])
                for kk in range(3):
                    k.stt(xc, xr[:, kk:kk + NT], chv[:, kk, c:c + 1], xc, ALU.mult, ALU.add)
                if hf == 0:
                    k.copy(xr[:, 0:3], xr[:, NT:NT + 3])
                k.act(xcb, xc, AF.Copy)

            def st_G(c, hf):
                par = (c * 2 + hf) % 2
                xcb, Ab, Gb = xcb2[par], Ab2[par], Gb2[par]
                for gi, buf in enumerate([Ab, Gb]):
                    for blk in range(2):
                        b = nb()
                        k.mm(PS[:, b, :], Wbd[:, c, gi, :], xcb[:, blk * 512:(blk + 1) * 512])
                        k.act(buf[:, blk * 512:(blk + 1) * 512], PS[:, b, :], AF.Tanh,
                              bias=bh[:, gi, c:c + 1], scale=0.5)

            def st_B(c, hf):
                par = (c * 2 + hf) % 2
                xc, Ab, Gb, Mb = xc2[par], Ab2[par], Gb2[par], Mb2[par]
                k.act(Mb, Ab, AF.Exp, bias=c1f[:, c:c + 1], scale=c1f[:, c:c + 1])
                k.act(Ab, Ab, AF.Exp, bias=c1h[:, c:c + 1], scale=c1h[:, c:c + 1])
                k.act(Mb, Mb, AF.Sqrt, bias=1.0, scale=-1.0)
                k.stt(Gb, Gb, 1.0, Mb, ALU.add, ALU.mult)
                k.stt(Gb, Gb, 0.5, xc, ALU.mult, ALU.mult)
                if hf == 0:
                    k.scan(Mb, Ab, Gb, 0.0)
                    k.tt(stt_[:, c:c + 1], Mb[:, NT - 1:NT], flg, ALU.mult)
                else:
                    k.scan(Mb, Ab, Gb, stt_[:, c:c + 1])

            def st_Yact(c, hf):
                if hf == 0:
                    return
                Mb = Mb2[(c * 2 + hf) % 2]
                for blk, b in enumerate(ybanks[(c, hf)]):
                    k.act(Yb[:, blk * 512:(blk + 1) * 512], PS[:, b, :], AF.Gelu_apprx_tanh)
                    held.discard(b)
                k.tt(y_a[:, c, :], Yb, Mb, ALU.mult)

            chains = [(c, hf) for c in range(8) for hf in range(2)]
            NCH = len(chains)
            st_Z(*chains[0])
            for t in range(NCH + 1):
                if t + 1 < NCH:
                    st_Z(*chains[t + 1])
                if t >= 1:
                    st_Ymm(*chains[t - 1])
                if t < NCH:
                    st_E(*chains[t])
                    st_G(*chains[t])
                if t >= 1:
                    st_B(*chains[t - 1])
                    st_Yact(*chains[t - 1])
            dump("y_a", y_a)

            P0 = view(TT + 0, [128, 16 + NT], F32)
            P1 = view(TT + 4160, [128, 16 + NT], F32)
            P2 = view(TT + 8320, [128, 16 + NT], F32)
            dbf = view(TT + 12480, [128, 2, NT], BF16)
            wpl = view(TT + 16576, [128, 4, 2, 256], BF16)
            k.dma(wpl, dr["w_pool"].rearrange("g (kc p) d -> p g kc d", p=128), q="pool")
            wp = None
            L = 16 + NT
            for pc in range(8):
                cc = pc % 2
                g = pc // 2
                w = 2 << g
                if cc == 0:
                    wp = wload(w_in_v[:, :, 2048 + pc * 128:2048 + pc * 128 + 256], [128, 16, 256])
                b = nb()
                for kc in range(KC):
                    k.mm(PS[:, b, 0:16], wp[:, kc, cc * 128:(cc + 1) * 128], hTp[:, kc, NT - 16:NT],
                         start=(kc == 0), stop=(kc == KC - 1))
                k.act(P0[:, 0:16], PS[:, b, 0:16], AF.Identity)
                for blk in range(2):
                    b = nb()
                    for kc in range(KC):
                        k.mm(PS[:, b, :], wp[:, kc, cc * 128:(cc + 1) * 128], hT[:, kc, blk * 512:(blk + 1) * 512],
                             start=(kc == 0), stop=(kc == KC - 1))
                    k.act(P0[:, 16 + blk * 512:16 + (blk + 1) * 512], PS[:, b, :], AF.Identity)
                cur, oth = P0, [P1, P2]
                sh = 1
                for st in range(g + 1):
                    dst = oth[st % 2]
                    lo = 2 * sh - 1
                    k.tt(dst[:, lo:L], cur[:, lo:L], cur[:, lo - sh:L - sh], ALU.add)
                    cur = dst
                    sh *= 2
                k.stt(dbf[:, cc, :], cur[:, 16:L], 1.0 / w, P0[:, 16:L], ALU.mult, ALU.subtract)
                k.tt(tmp16, cur[:, 16:32], invc[:, g * 16:(g + 1) * 16], ALU.mult)
                k.tt(dbf[:, cc, 0:16], tmp16, P0[:, 16:32], ALU.subtract)
                if cc == 1:
                    for oc in range(2):
                        for blk in range(2):
                            b = nb()
                            for kc2 in range(2):
                                k.mm(PS[:, b, :], wpl[:, g, kc2, oc * 128:(oc + 1) * 128], dbf[:, kc2, blk * 512:(blk + 1) * 512],
                                     start=(kc2 == 0), stop=(kc2 == 1))
                            k.act(y_b[:, g * 2 + oc, blk * 512:(blk + 1) * 512], PS[:, b, :], AF.Identity,
                                  scale=chv[:, 8, g * 2 + oc:g * 2 + oc + 1])
            dump("y_b", y_b)

            sA = [view(TT + i * 2 * KB, [128, 512], F32) for i in range(2)]
            sB = [view(TT + 4 * KB + i * 2 * KB, [128, 512], F32) for i in range(2)]
            t1 = [view(TT + 8 * KB + i * 2 * KB, [128, 512], F32) for i in range(2)]
            wa_v = dr["w_branch_a"].rearrange("(kc p) c -> p kc c", p=128)
            wb_v = dr["w_branch_b"].rearrange("(kc p) c -> p kc c", p=128)
            ga = gb = wA = wB = None
            it = 0
            for oc in range(16):
                if oc % 4 == 0:
                    wA = wload(wa_v[:, :, oc * 128:oc * 128 + 512], [128, 8, 512])
                    wB = wload(wb_v[:, :, oc * 128:oc * 128 + 512], [128, 8, 512])
                if oc % 2 == 0:
                    ga = wload(w_in_v[:, :, 3072 + oc * 128:3072 + oc * 128 + 256], [128, 16, 256])
                    gb = wload(w_in_v[:, :, 5120 + oc * 128:5120 + oc * 128 + 256], [128, 16, 256])
                c2 = oc % 2
                c4 = oc % 4
                for blk in range(2):
                    bs = slice(blk * 512, (blk + 1) * 512)
                    i2 = it % 2
                    it += 1
                    b1 = nb()
                    for kc in range(KC):
                        k.mm(PS[:, b1, :], ga[:, kc, c2 * 128:(c2 + 1) * 128], hT[:, kc, bs], start=(kc == 0), stop=(kc == KC - 1))
                    k.act(sA[i2], PS[:, b1, :], AF.Sigmoid)
                    b2 = nb()
                    for kc in range(8):
                        k.mm(PS[:, b2, :], wA[:, kc, c4 * 128:(c4 + 1) * 128], y_a[:, kc, bs], start=(kc == 0), stop=(kc == 7))
                    k.tt(sA[i2], sA[i2], PS[:, b2, :], ALU.mult)
                    b3 = nb()
                    for kc in range(KC):
                        k.mm(PS[:, b3, :], gb[:, kc, c2 * 128:(c2 + 1) * 128], hT[:, kc, bs], start=(kc == 0), stop=(kc == KC - 1))
                    k.act(sB[i2], PS[:, b3, :], AF.Sigmoid)
                    b4 = nb()
                    for kc in range(8):
                        k.mm(PS[:, b4, :], wB[:, kc, c4 * 128:(c4 + 1) * 128], y_b[:, kc, bs], start=(kc == 0), stop=(kc == 7))
                    k.tt(sB[i2], sB[i2], PS[:, b4, :], ALU.mult)
                    k.tt(uT[:, oc, bs], sA[i2], sB[i2], ALU.add)
            dump("uT", uT)

            xrl1 = view(TT + 28 * KB, [128, D], F32)
            wo_v = dr["w_out"].rearrange("(kc p) c -> p kc c", p=128)
            wo_all = [[wload(wo_v[:, h * 8:(h + 1) * 8, db * 512:(db + 1) * 512], [128, 8, 512]) for h in range(2)]
                      for db in range(4)]
            gbc2 = view(TT + 0, [128, D], F32)
            xn32s = [view(TT + 8 * KB, [128, D], F32)] * 2
            ht32T = view(TT + 16 * KB, [128, 16, 128], F32)
            k.dma(gbc2, dr["norm2_g"].partition_broadcast(128), q="sp")

            sqjunk2 = view(TT + 24 * KB, [128, D], BF16)
            rtbanks, rtb2 = {}, {}

            def rt_s0(i):
                k.act(sqjunk2, x1v[:, i, :], AF.Square, accum_out=ss[:, i:i + 1])

            def rt_s1(i):
                k.ts(tmpv[:, i:i + 1], ss[:, i:i + 1], 1.0 / D, EPS, ALU.mult, ALU.add)

            def rt_s2(i):
                k.act(tmpv[:, i:i + 1], tmpv[:, i:i + 1], AF.Sqrt)

            def rt_s3(i):
                k.recip(rstd[:, i:i + 1], tmpv[:, i:i + 1])
                k.stt(xn32s[i % 2], x1v[:, i, :], rstd[:, i:i + 1], gbc2, ALU.mult, ALU.mult)

            def rt_s4(i):
                xn32 = xn32s[i % 2]
                bl = []
                for q in range(4):
                    b = nb()
                    held.add(b)
                    bl.append(b)
                    for j in range(4):
                        kc = q * 4 + j
                        k.tr(PS[:, b, j * 128:(j + 1) * 128], xn32[:, kc * 128:(kc + 1) * 128], ident_f)
                rtbanks[i] = bl

            def rt_s5(i):
                for q, b in enumerate(rtbanks[i]):
                    srcp = PS[:, b, :].rearrange("p (a b) -> p a b", a=4)
                    if q % 2 == 0:
                        k.act(ht32T[:, q * 4:(q + 1) * 4, :], srcp, AF.Identity)
                    else:
                        k.copy(ht32T[:, q * 4:(q + 1) * 4, :], srcp)
                    held.discard(b)
                b = nb()
                held.add(b)
                rtb2[i] = b
                for kc in range(KC):
                    k.mm(PS[:, b, 0:20], ht32T[:, kc, :], wr[:, kc, :], start=(kc == 0), stop=(kc == KC - 1))

            def rt_s6(i):
                b = rtb2[i]
                k.tt(lg[:, i, :], PS[:, b, 0:20], rb, ALU.add)
                held.discard(b)


            def wout_tile(i):
                k.dma(xrl1, dr["xm"][i * 128:(i + 1) * 128, :], q="sp", key="xrl1")
                for db in range(4):
                    ds_ = slice(db * 512, (db + 1) * 512)
                    b = nb()
                    for kc in range(KC):
                        k.mm(PS[:, b, :], uT[:, kc, i * 128:(i + 1) * 128], wo_all[db][kc // 8][:, kc % 8, :],
                             start=(kc == 0), stop=(kc == KC - 1))
                    k.tt(x1v[:, i, ds_], xrl1[:, ds_], PS[:, b, :], ALU.add)

            pipeline(8, [wout_tile, rt_s0, rt_s1, rt_s2, rt_s3, rt_s4, rt_s5, rt_s6])
            x1s_w = []
            for i in range(8):
                x1s_w.append(k.dma(x1s[i * 128:(i + 1) * 128, :], x1v[:, i, :], q="sp"))
            dump("x1", x1v)

            dmyA = view(CC + 7072, [128, 1], F32)
            dmyD = view(CC + 7088, [128, 1], F32)
            k.memset(dmyA, 0.0)
            k.memset(dmyD, 0.0)
            S.dummy["pe"] = lambda e: e.matmul(PS[:, 7, 0:2], ident_bf, ident_bf[:, 0:2], start=True, stop=True)
            S.dummy["act"] = lambda e: e.activation(dmyA, dmyA, AF.Identity)
            S.dummy["dve"] = lambda e: e.memset(dmyD, 0.0)
            Lf = view(CC + 7104, [128, 128], F32)
            Ltri = view(CC + 7616, [128, 128], BF16)
            ones_bf = view(CC + 7872, [128, 128], BF16)
            siota = view(CC + 8128, [128, 8], F32)
            jv = view(CC + 8160, [128, 8], F32)
            oh_bf = view(CC + 8192, [128, 32], BF16)
            tot_s = view(CC + 8256, [128, 8, 4], F32)
            wit_s = view(CC + 8384, [128, 8, 4], F32)
            tpre = view(CC + 8512, [128, 8, 4], F32)
            ngv = view(CC + 8640, [128, 4], F32)
            offv = view(CC + 8656, [128, 4], F32)
            endv = view(CC + 8672, [128, 4], F32)
            posv = view(CC + 8688, [128, 8], F32)
            fl_a = view(CC + 8720, [128, 4, 8], F32)
            fl_b = view(CC + 8848, [128, 4, 8], F32)
            fl_i = view(CC + 8976, [128, 4, 8], I32)
            cmb_hl = view(CC + 9104, [128, 8, 32], BF16)
            cmb_t = view(CC + 9616, [128, 8, 16], F32)
            comb_s = view(CC + 10128, [128, 8, 16], F32)
            iint = view(CC + 10640, [128, 16], I32)
            umask = view(CC + 10832, [128, 4, 8], F32)
            flU = view(CC + 10960, [128, 4], F32)
            flU_i = view(CC + 10976, [128, 4], I32)
            LIKELY = [(0, 3), (1, 5), (3, 7), (5, 8)]
            c32 = view(CC + 10704, [128, 32], F32)
            k.memset(Lf, 1.0)
            S.add("pool", lambda e: e.affine_select(out=Lf, in_=Lf, pattern=[[1, 128]], compare_op=ALU.is_gt,
                                                    fill=0.0, base=0, channel_multiplier=-1), reads=[Lf], writes=[Lf])
            k.copy(Ltri, Lf)
            k.memset(Lf, 1.0)
            k.memset(ones_bf, 1.0)
            S.add("pool", lambda e: e.iota(iint[:, 0:8], pattern=[[128, 8]], base=0, channel_multiplier=1), writes=[iint[:, 0:8]])
            S.add("pool", lambda e: e.iota(iint[:, 8:16], pattern=[[128, 8]], base=0, channel_multiplier=0), writes=[iint[:, 8:16]])
            k.copy(siota, iint[:, 0:8])
            k.copy(jv, iint[:, 8:16])

            dump("lg", lg)
            lgG = lg[:, :, 0:4]
            lgE4 = lg[:, :, 4:20].rearrange("p t (g j) -> p t j g", g=4)
            mx, sg_, gw, m1, m2, se, gw2 = rs1[0], rs1[1], rs1[2], rs1[3], rs1[4], rs1[5], rs1[6]
            oh, eg, lsel, msk, le2, sel, ee, wt4 = rsc[0], rsc[1], rsc[2], rsc[3], rsc[4], rsc[5], rsc[6], rsc[7]
            basev, posg = rsc[8], rsc[9]

            def bc3(v):
                return v.unsqueeze(2).to_broadcast([128, 8, 4])
            k.reduce(mx, lgG, ALU.max)
            k.tt(oh, lgG, bc3(mx), ALU.is_equal)
            k.tt(eg, lgG, bc3(mx), ALU.subtract)
            k.act(eg, eg, AF.Exp)
            k.reduce(sg_, eg, ALU.add)
            k.recip(gw, sg_)
            k.tt(prod, lgE4, oh.unsqueeze(2).to_broadcast([128, 8, 4, 4]), ALU.mult)
            k.reduce(lsel, prod, ALU.add)
            k.reduce(m1, lsel, ALU.max)
            k.tt(msk, lsel, bc3(m1), ALU.is_equal)
            k.stt(le2, msk, -1e30, lsel, ALU.mult, ALU.add)
            k.reduce(m2, le2, ALU.max)
            k.tt(sel, lsel, bc3(m2), ALU.is_ge)
            k.tt(ee, lsel, bc3(m1), ALU.subtract)
            k.act(ee, ee, AF.Exp)
            k.tt(ee, ee, sel, ALU.mult)
            k.reduce(se, ee, ALU.add)
            k.recip(gw2, se)
            k.tt(gw2, gw2, gw, ALU.mult)
            k.tt(wt4, ee, bc3(gw2), ALU.mult)
            comb4 = comb.rearrange("p t (g j) -> p t g j", g=4)
            k.tt(comb4, oh.unsqueeze(3).to_broadcast([128, 8, 4, 4]), wt4.unsqueeze(2).to_broadcast([128, 8, 4, 4]), ALU.mult)
            dump("comb", comb)

            k.copy(oh_bf, oh.rearrange("p t g -> p (t g)"))
            b = nb()
            k.mm(PS[:, b, 0:32], ones_bf, oh_bf)
            k.copy(tot_s.rearrange("p t g -> p (t g)"), PS[:, b, 0:32])
            b = nb()
            k.mm(PS[:, b, 0:32], Ltri, oh_bf)
            k.copy(wit_s.rearrange("p t g -> p (t g)"), PS[:, b, 0:32])
            k.memset(tpre[:, 0, :], 0.0)
            for i in range(1, 8):
                k.tt(tpre[:, i, :], tpre[:, i - 1, :], tot_s[:, i - 1, :], ALU.add)
            k.tt(ngv, tpre[:, 7, :], tot_s[:, 7, :], ALU.add)
            k.memset(offv[:, 0:1], 0.0)
            for g in range(1, 4):
                k.tt(offv[:, g:g + 1], offv[:, g - 1:g], ngv[:, g - 1:g], ALU.add)
            k.tt(endv, offv, ngv, ALU.add)
            k.tt(basev, tpre, offv.unsqueeze(1).to_broadcast([128, 8, 4]), ALU.add)
            k.tt(basev, basev, wit_s, ALU.add)
            k.tt(posg, basev, oh, ALU.mult)
            k.reduce(posv, posg, ALU.add)
            offb = offv.unsqueeze(2).to_broadcast([128, 4, 8])
            endb = endv.unsqueeze(2).to_broadcast([128, 4, 8])
            jvb = jv.unsqueeze(1).to_broadcast([128, 4, 8])
            k.tt(fl_a, offb, jvb, ALU.subtract)
            k.ts(fl_a, fl_a, 128.0, None, ALU.is_lt)
            k.tt(fl_b, endb, jvb, ALU.subtract)
            k.ts(fl_b, fl_b, 0.0, None, ALU.is_gt)
            k.tt(fl_a, fl_a, fl_b, ALU.mult)
            k.copy(fl_i, fl_a)
            k.memset(umask.rearrange("p g j -> p (g j)"), 1.0)
            for g in range(4):
                lo_, hi_ = LIKELY[g]
                k.memset(umask[:, g, lo_:hi_], 0.0)
            k.tt(umask, umask, fl_a, ALU.mult)
            k.reduce(flU, umask, ALU.max)
            k.copy(flU_i, flU)
            dump("posv", posv)
            dump("fl_a", fl_a)

            iota_i = view(A0, [128, NT], I32)
            iota_s = view(A0 + 4 * KB, [128, NT], F32)
            Pm = view(TT + 0, [128, 8, NT], BF16)
            S.add("pool", lambda e: e.iota(iota_i, pattern=[[1, NT]], base=0, channel_multiplier=0), writes=[iota_i])
            k.copy(iota_s, iota_i)
            for i in range(8):
                k.ts(Pm[:, i, :], iota_s, posv[:, i:i + 1], None, ALU.is_equal)
            k.copy(cmb_hl[:, :, 0:16], comb)
            k.tt(cmb_t, comb, cmb_hl[:, :, 0:16], ALU.subtract)
            k.copy(cmb_hl[:, :, 16:32], cmb_t)
            for j in range(8):
                b = nb()
                for i in range(8):
                    k.mm(PS[:, b, 0:32], Pm[:, i, j * 128:(j + 1) * 128], cmb_hl[:, i, :], start=(i == 0), stop=(i == 7))
                k.copy(c32, PS[:, b, 0:32])
                k.tt(comb_s[:, j, :], c32[:, 0:16], c32[:, 16:32], ALU.add)
            dump("comb_s", comb_s)

            ht_tok = view(TT + 16 * KB, [128, 8, 1024], BF16)
            gbh = view(TT + 32 * KB, [128, 1024], F32)
            htS = hTp
            ev = 0
            for hh in range(2):
                fsl = slice(hh * 1024, (hh + 1) * 1024)
                k.dma(gbh, dr["norm2_g"][fsl].partition_broadcast(128), q="sp", key="gbh")
                for i in range(8):
                    k.stt(ht_tok[:, i, :], x1v[:, i, fsl], rstd[:, i:i + 1], gbh, ALU.mult, ALU.mult)
                for kc8 in range(8):
                    kc = hh * 8 + kc8
                    for sb in range(2):
                        b = nb()
                        for i in range(8):
                            k.mm(PS[:, b, :], ht_tok[:, i, kc8 * 128:(kc8 + 1) * 128], Pm[:, i, sb * 512:(sb + 1) * 512],
                                 start=(i == 0), stop=(i == 7))
                        dst = htS[:, kc, sb * 512:(sb + 1) * 512]
                        if ev % 2 == 0:
                            k.act(dst, PS[:, b, :], AF.Identity)
                        else:
                            k.copy(dst, PS[:, b, :])
                        ev += 1
            dump("htS", htS)

            ysum = x1v
            for j in range(8):
                k.memset(ysum[:, j, :], 0.0)

            slb = [view(TT + i * 2 * KB, [128, 512], F32) for i in range(2)]
            atok = [view(TT + 4 * KB + i * KB, [128, 512], BF16) for i in range(2)]
            aT_all = [view(TT + 6 * KB, [128, 8, 4, 128], BF16)] * 2
            ring_addrs.extend([TT + 16 * KB, TT + 24 * KB])
            bi = 0
            for g in range(4):
                for en in ("pe", "act", "dve"):
                    for j in range(8):
                        S.flagload(en, "j%d" % j, fl_i[0:1, g, j:j + 1])
                    S.flagload(en, "U", flU_i[0:1, g:g + 1])
                lo_, hi_ = LIKELY[g]
                jorder = list(range(lo_, hi_)) + [j for j in range(8) if not (lo_ <= j < hi_)]
                for e4 in range(4):
                    e = g * 4 + e4
                    wgv = dr["w_e_gate"][e].rearrange("(kc p) f -> p kc f", p=128)
                    wuv = dr["w_e_up"][e].rearrange("(kc p) f -> p kc f", p=128)
                    wdv = dr["w_e_down"][e].rearrange("(fc p) d -> p fc d", p=128)
                    wgs = [wload(wgv[:, h * 8:(h + 1) * 8, :], [128, 8, 512]) for h in range(2)]
                    wus = [wload(wuv[:, h * 8:(h + 1) * 8, :], [128, 8, 512]) for h in range(2)]
                    wds = [wload(wdv[:, :, h * 1024:(h + 1) * 1024], [128, 4, 1024]) for h in range(2)]
                    aTe = aT_all[e % 2]
                    in_outer = False
                    for j in jorder:
                        unlikely = not (lo_ <= j < hi_)
                        if unlikely and not in_outer:
                            S.cond_begin(("Ua", g, e4), "U")
                            in_outer = True
                        S.cond_begin((g, e4, j, "a"), "j%d" % j)
                        js = slice(j * 128, (j + 1) * 128)
                        bg = nb()
                        for kc in range(KC):
                            k.mm(PS[:, bg, :], htS[:, kc, js], wgs[kc // 8][:, kc % 8, :], start=(kc == 0), stop=(kc == KC - 1))
                        bu = nb()
                        for kc in range(KC):
                            k.mm(PS[:, bu, :], htS[:, kc, js], wus[kc // 8][:, kc % 8, :], start=(kc == 0), stop=(kc == KC - 1))
                        sl = slb[bi % 2]
                        at = atok[bi % 2]
                        bi += 1
                        k.act(sl, PS[:, bg, :], AF.Silu)
                        k.tt(at, sl, PS[:, bu, :], ALU.mult)
                        bt = nb()
                        pbt = psb(bt)
                        for fc in range(4):
                            k.tr(pbt[:, fc * 128:(fc + 1) * 128], at[:, fc * 128:(fc + 1) * 128], ident_bf)
                        k.act(aTe[:, j, :, :], pbt[:, 0:512].rearrange("p (a b) -> p a b", a=4), AF.Identity)
                        S.cond_end()
                    if in_outer:
                        S.cond_end()
                    in_outer = False
                    for j in jorder:
                        unlikely = not (lo_ <= j < hi_)
                        if unlikely and not in_outer:
                            S.cond_begin(("Ub", g, e4), "U")
                            in_outer = True
                        S.cond_begin((g, e4, j, "b"), "j%d" % j)
                        for db in range(4):
                            b = nb()
                            for fc in range(4):
                                k.mm(PS[:, b, :], aTe[:, j, fc, :], wds[db // 2][:, fc, (db % 2) * 512:(db % 2 + 1) * 512],
                                     start=(fc == 0), stop=(fc == 3))
                            ys = ysum[:, j, db * 512:(db + 1) * 512]
                            k.stt(ys, PS[:, b, :], comb_s[:, j, e:e + 1], ys, ALU.mult, ALU.add)
                        S.cond_end()
                    if in_outer:
                        S.cond_end()
            del ring_addrs[NSLOT:]
            dump("ysum", ysum)

            posrow = view(TT + 8 * KB, [128, NT], F32)
            dg = view(TT + 12 * KB, [128, 128], F32)
            PT = view(TT + 16 * KB, [128, 8, NT], BF16)
            yh = view(A0, [128, 8, 1024], BF16)
            yl = view(A0 + 16 * KB, [128, 8, 1024], BF16)
            xn32 = xn32s[0]
            ytmp = view(TT + 32 * KB, [128, 1024], F32)
            for hb in range(2):
                b = nb()
                for i4 in range(4):
                    i = hb * 4 + i4
                    k.ts(dg, ident_f, posv[:, i:i + 1], None, ALU.mult)
                    k.mm(PS[:, b, i4 * 128:(i4 + 1) * 128], Lf, dg)
                k.copy(posrow[:, hb * 512:(hb + 1) * 512], PS[:, b, :])
            for j in range(8):
                k.ts(PT[:, j, :], posrow, siota[:, j:j + 1], None, ALU.is_equal)
            cv = 0
            yhs = [yh, yl]
            for dh in range(2):
                dsl = slice(dh * 1024, (dh + 1) * 1024)
                yh = yhs[dh]
                for j in range(8):
                    if j % 2 == 0:
                        k.act(yh[:, j, :], ysum[:, j, dsl], AF.Copy)
                    else:
                        k.copy(yh[:, j, :], ysum[:, j, dsl])
                for i in range(8):
                    dst = x1v[:, i, dsl]
                    src = x1s[i * 128:(i + 1) * 128, dsl]
                    S.add("sp", (lambda e, dst=dst, src=src: e.dma_start(out=dst, in_=src)), reads=[], writes=[dst],
                          dma_key="x1r%d_%d" % (dh, i), extra_deps=[x1s_w[i]])
                for i in range(8):
                    for d2 in range(2):
                        b = nb()
                        n = 0
                        for j in range(8):
                            k.mm(PS[:, b, :], PT[:, j, i * 128:(i + 1) * 128], yh[:, j, d2 * 512:(d2 + 1) * 512],
                                 start=(j == 0), stop=(j == 7))
                        dcol = slice(dh * 1024 + d2 * 512, dh * 1024 + (d2 + 1) * 512)
                        k.tt(x1v[:, i, dcol], x1v[:, i, dcol], PS[:, b, :], ALU.add)
            dump("x2", x1v)

            gbc3 = view(TT + 0, [128, D], F32)
            xnb3s = [view(TT + 8 * KB + i * 4 * KB, [128, D], BF16) for i in range(2)]
            pT = view(TT + 16 * KB, [128, 2, NT], BF16)
            pl = [view(TT + 20 * KB + i * KB, [128, 256], F32) for i in range(2)]
            pbf = view(TT + 22 * KB, [128, 256], BF16)
            sg3 = [view(TT + 23 * KB + i * 2 * KB, [128, 512], F32) for i in range(2)]
            sqjunk3 = view(TT + 27 * KB, [128, D], BF16)
            ost = [view(TT + 8 * KB + i * 8 * KB, [128, D], F32) for i in range(2)]
            k.dma(gbc3, dr["norm_ple_g"].partition_broadcast(128), q="sp")
            for i in range(8):
                k.dma(pl[i % 2], dr["pm"][i * 128:(i + 1) * 128, :], q="sp", key="pl%d" % (i % 2))
                k.copy(pbf, pl[i % 2])
                b = nb()
                pb = psb(b)
                for j in range(2):
                    k.tr(pb[:, j * 128:(j + 1) * 128], pbf[:, j * 128:(j + 1) * 128], ident_bf)
                k.copy(pT[:, :, i * 128:(i + 1) * 128], pb[:, 0:256].rearrange("p (a b) -> p a b", a=2))
            plbanks = {}

            def pl_s0(i):
                k.act(sqjunk3, x1v[:, i, :], AF.Square, accum_out=ss[:, i:i + 1])

            def pl_s1(i):
                k.ts(tmpv[:, i:i + 1], ss[:, i:i + 1], 1.0 / D, EPS, ALU.mult, ALU.add)

            def pl_s2(i):
                k.act(tmpv[:, i:i + 1], tmpv[:, i:i + 1], AF.Sqrt)

            def pl_s3(i):
                k.recip(rstd[:, i:i + 1], tmpv[:, i:i + 1])
                k.stt(xnb3s[i % 2], x1v[:, i, :], rstd[:, i:i + 1], gbc3, ALU.mult, ALU.mult)

            def pl_s4(i):
                xnb3 = xnb3s[i % 2]
                bl = []
                for h in range(2):
                    b = nb()
                    held.add(b)
                    bl.append(b)
                    pb = psb(b)
                    for j in range(8):
                        kc = h * 8 + j
                        k.tr(pb[:, j * 128:(j + 1) * 128], xnb3[:, kc * 128:(kc + 1) * 128], ident_bf)
                plbanks[i] = bl

            def pl_s5(i):
                for h, b in enumerate(plbanks[i]):
                    dst = hpT[:, h * 8:(h + 1) * 8, i * 128:(i + 1) * 128]
                    srcp = psb(b).rearrange("p (a b) -> p a b", a=8)
                    if h == 0:
                        k.act(dst, srcp, AF.Identity)
                    else:
                        k.copy(dst, srcp)
                    held.discard(b)

            pipeline(8, [pl_s0, pl_s1, pl_s2, pl_s3, pl_s4, pl_s5])
            wpp_v = dr["w_ple_proj"].rearrange("(kc p) d -> p kc d", p=128)
            wpg_v = dr["w_ple_gate"].rearrange("(kc p) c -> p kc c", p=128)
            it = 0
            for db in range(4):
                ds_ = slice(db * 512, (db + 1) * 512)
                wpg = [wload(wpg_v[:, h * 8:(h + 1) * 8, ds_], [128, 8, 512]) for h in range(2)]
                wpp = wload(wpp_v[:, :, ds_], [128, 2, 512])
                for i in range(8):
                    ts_ = slice(i * 128, (i + 1) * 128)
                    b1 = nb()
                    for kc in range(KC):
                        k.mm(PS[:, b1, :], hpT[:, kc, ts_], wpg[kc // 8][:, kc % 8, :], start=(kc == 0), stop=(kc == KC - 1))
                    b2 = nb()
                    for kc2 in range(2):
                        k.mm(PS[:, b2, :], pT[:, kc2, ts_], wpp[:, kc2, :], start=(kc2 == 0), stop=(kc2 == 1))
                    sgt = sg3[it % 2]
                    it += 1
                    k.act(sgt, PS[:, b1, :], AF.Sigmoid)
                    k.tt(sgt, sgt, PS[:, b2, :], ALU.mult)
                    k.tt(x1v[:, i, ds_], x1v[:, i, ds_], sgt, ALU.add)
            k.dma(gbc3, dr["final_norm_g"].partition_broadcast(128), q="sp")
            def fn_s0(i):
                k.act(sqjunk3, x1v[:, i, :], AF.Square, accum_out=ss[:, 8 + i:9 + i])

            def fn_s1(i):
                k.ts(tmpv[:, 8 + i:9 + i], ss[:, 8 + i:9 + i], 1.0 / D, EPS, ALU.mult, ALU.add)

            def fn_s2(i):
                k.act(tmpv[:, 8 + i:9 + i], tmpv[:, 8 + i:9 + i], AF.Sqrt)

            def fn_s3(i):
                k.recip(rstd[:, 8 + i:9 + i], tmpv[:, 8 + i:9 + i])
                k.stt(ost[i % 2], x1v[:, i, :], rstd[:, 8 + i:9 + i], gbc3, ALU.mult, ALU.mult)

            def fn_s4(i):
                stores.append(k.dma(out[i * 128:(i + 1) * 128, :], ost[i % 2], q="sp", key="ost%d" % (i % 2)))

            pipeline(8, [fn_s0, fn_s1, fn_s2, fn_s3, fn_s4])

        except _Stop:
            pass
        S.add("sp", lambda e: e.nop(), extra_deps=stores + list(dbg_out.values()))
        build_program.stats = S.stats()
        S.emit()
    return nc


_CACHE = {}


def _host_consts():
    flags = []
    invs = []
    wins = (2, 4, 8, 16)
    for core in range(8):
        odd = core % 2
        flags.append(np.full((128, 1), float(odd), np.float32))
        iv = np.zeros((128, 64), np.float32)
        for g, w in enumerate(wins):
            for t in range(16):
                tg = t + (NT if odd else 0)
                iv[:, g * 16 + t] = 1.0 / min(tg + 1, w)
        invs.append(iv)
    return flags, invs


def kernel(**inputs):
    debug = tuple(inputs.pop("_debug", ()))
    x = np.ascontiguousarray(inputs["x"], dtype=np.float32)
    p = np.ascontiguousarray(inputs["p"], dtype=np.float32)[0]
    key = ("prog", debug)
    if key not in _CACHE:
        _CACHE[key] = build_program(debug)
    nc = _CACHE[key]
    w = {}
    for nm, shp in W_SPECS:
        a = np.asarray(inputs[nm], dtype=np.float32)
        w[nm] = np.ascontiguousarray(a.reshape(shp))
    flags, invs = _host_consts()
    zeros = np.zeros((NT, D), np.float32)
    in_maps = []
    for core in range(8):
        b, half = core // 2, core % 2
        m = dict(w)
        m["xm"] = np.ascontiguousarray(x[b, half * NT:(half + 1) * NT])
        m["xp"] = np.ascontiguousarray(x[b, 0:NT]) if half == 1 else zeros
        m["pm"] = np.ascontiguousarray(p[b, half * NT:(half + 1) * NT])
        m["flag"] = flags[core]
        m["invcnt"] = invs[core]
        in_maps.append(m)
    res = run_bass_kernel_spmd(nc, in_maps, core_ids=list(range(8)))
    outp = np.empty((4, 2048, D), np.float32)
    for core in range(8):
        b, half = core // 2, core % 2
        outp[b, half * NT:(half + 1) * NT] = res.results[core]["out"]
    if debug:
        kernel.last_debug = [{kk: vv for kk, vv in r.items() if kk.startswith("dbg_")} for r in res.results]
    return outp
```

```python
import numpy as np
import concourse.bass as bass
import concourse.mybir as mybir
from contextlib import ExitStack

F32 = mybir.dt.float32
BF16 = mybir.dt.bfloat16
I32 = mybir.dt.int32
AF = mybir.ActivationFunctionType
ALU = mybir.AluOpType
AX = mybir.AxisListType
_DSZ = {F32: 4, BF16: 2, mybir.dt.int32: 4, mybir.dt.uint32: 4}


def _region(ap):
    tname = type(ap.tensor).__name__
    if "DRam" in tname:
        return None
    key = "PS" if "PSum" in tname else ap.tensor.name
    sz = _DSZ[ap.dtype]
    dims = ap.ap
    pstep, pcnt = dims[0]
    off = int(ap.offset)
    if pstep > 0:
        p0 = off // pstep
        f0 = off % pstep
    else:
        p0, f0 = 0, off
    free = [(s, c) for (s, c) in dims[1:] if c > 1]
    if key == "PS":
        ext = 1
        for s_, c_ in free:
            ext += (c_ - 1) * abs(s_)
        lo = (f0 * sz) // 2048 * 2048
        hi = -(-((f0 + ext) * sz) // 2048) * 2048
        return key, 0, 128, [(lo, hi)]
    if not free:
        return key, p0, p0 + pcnt, [(f0 * sz, (f0 + 1) * sz)]
    ls, lc = free[-1]
    outer = free[:-1]
    n_outer = 1
    for s, c in outer:
        n_outer *= c
    run = (lc - 1) * abs(ls) + 1
    if n_outer <= 64:
        offs = [0]
        for s, c in outer:
            offs = [o + i * s for o in offs for i in range(c)]
        ivs = sorted((f0 + o) for o in offs)
        out = []
        for o in ivs:
            lo, hi = o * sz, (o + run) * sz
            if out and lo <= out[-1][1]:
                out[-1] = (out[-1][0], max(out[-1][1], hi))
            else:
                out.append((lo, hi))
        return key, p0, p0 + pcnt, out
    ext = run
    for s, c in outer:
        ext += (c - 1) * abs(s)
    return key, p0, p0 + pcnt, [(f0 * sz, (f0 + ext) * sz)]


def _iv_overlap(a, b):
    i = j = 0
    while i < len(a) and j < len(b):
        if a[i][1] <= b[j][0]:
            i += 1
        elif b[j][1] <= a[i][0]:
            j += 1
        else:
            return True
    return False


def _iv_covers(a, b):
    for lo, hi in b:
        ok = False
        for alo, ahi in a:
            if alo <= lo and hi <= ahi:
                ok = True
                break
        if not ok:
            return False
    return True


class Op:
    __slots__ = ("eng", "fn", "tl", "tpos", "waits", "need_inc", "semval", "done", "is_dma", "idx", "extra", "cond")


class Sched:
    ENGS = ("pe", "act", "dve", "pool", "sp")

    def __init__(self, nc):
        self.nc = nc
        self.ops = []
        self.by_eng = {e: [] for e in self.ENGS}
        self.eclock = {e: {} for e in self.ENGS}
        self.recs = {}
        self.tl_count = {}
        self.dma_group_total = {}
        self._cond = None
        self._cond_snap = None
        self.regs = {}
        self.dummy = {}

    def cond_begin(self, tag, regkey):
        if self._cond is None:
            self._cond = ()
            self._cond_snaps = []
        self._cond = self._cond + ((tag, regkey),)
        self._cond_snaps.append({e: dict(c) for e, c in self.eclock.items()})
        self._cond_snap = self._cond_snaps[0]

    def cond_end(self):
        self.eclock = self._cond_snaps.pop()
        self._cond = self._cond[:-1]
        if not self._cond:
            self._cond = None
            self._cond_snap = None

    def flagload(self, eng, key, flag_ap):
        def fn(e, eng=eng, key=key):
            if (eng, key) not in self.regs:
                self.regs[(eng, key)] = e.alloc_register("fl_%s_%s" % (eng, key))
            return e.reg_load(self.regs[(eng, key)], flag_ap)
        return self.add(eng, fn, reads=[flag_ap])

    def add(self, eng, fn, reads=(), writes=(), dma_key=None, extra_deps=()):
        op = Op()
        op.eng = eng
        op.fn = fn
        op.is_dma = dma_key is not None
        op.tl = ("dma", dma_key) if op.is_dma else eng
        op.tpos = self.tl_count.get(op.tl, 0) + 1
        self.tl_count[op.tl] = op.tpos
        op.need_inc = op.is_dma
        op.semval = None
        op.waits = []
        op.idx = len(self.ops)
        op.extra = None
        op.cond = self._cond if self._cond is not None else ()
        assert not (op.is_dma and op.cond)
        deps = list(extra_deps)
        accs = []
        for ap in reads:
            r = _region(ap)
            if r is not None:
                accs.append((r, False))
        for ap in writes:
            r = _region(ap)
            if r is not None:
                accs.append((r, True))
        for (key, p0, p1, ivs), is_w in accs:
            lst = self.recs.setdefault(key, [])
            for rec in lst:
                if not (rec[3] or is_w) and key != "PS":
                    continue
                if rec[1] <= p0 or p1 <= rec[0]:
                    continue
                if not _iv_overlap(rec[2], ivs):
                    continue
                d = rec[4]
                if d is op:
                    continue
                if (not op.is_dma) and (not d.is_dma) and d.eng == eng:
                    if eng == "pe":
                        continue
                    if not (rec[3] or is_w):
                        continue
                deps.append(d)
        clock = dict(self.eclock[eng])
        deps.sort(key=lambda d: -d.idx)
        for d in deps:
            if clock.get(d.tl, 0) >= d.tpos:
                continue
            op.waits.append(d)
            d.need_inc = True
            for k, v in d.done.items():
                if clock.get(k, 0) < v:
                    clock[k] = v
        self.eclock[eng] = clock
        if op.cond:
            op.done = dict(self._cond_snap[eng])
        else:
            op.done = dict(clock)
        op.done[op.tl] = op.tpos
        for (key, p0, p1, ivs), is_w in accs:
            lst = self.recs[key]
            if is_w:
                lst[:] = [r for r in lst if not (p0 <= r[0] and r[1] <= p1 and _iv_covers(ivs, r[2]))]
            else:
                lst[:] = [r for r in lst if not ((not r[3]) and r[4].tl == op.tl and r[0] == p0 and r[1] == p1 and r[2] == ivs)]
            lst.append([p0, p1, ivs, is_w, op])
        self.ops.append(op)
        self.by_eng[eng].append(op)
        return op

    def emit(self):
        nc = self.nc
        cnt = {}
        for op in self.ops:
            if op.is_dma:
                op.semval = 16 * op.tpos
            elif op.need_inc:
                cnt[op.tl] = cnt.get(op.tl, 0) + 1
                op.semval = cnt[op.tl]
        for k, v in cnt.items():
            assert v < 60000, (k, v)
        tls = sorted({op.tl for op in self.ops if op.need_inc}, key=str)
        with ExitStack() as es:
            sems = {}
            for i, tl in enumerate(tls):
                nm = "s_" + ("_".join(str(x) for x in tl) if isinstance(tl, tuple) else tl)
                sems[tl] = es.enter_context(nc.semaphore(nm))
            block = es.enter_context(nc.Block())

            def run(engname):
                def emit_op(eng, op):
                    for d in op.waits:
                        eng.wait_ge(sems[d.tl], d.semval)
                    ins = op.fn(eng)
                    if op.need_inc:
                        ins.then_inc(sems[op.tl], 16 if op.is_dma else 1)

                def emit_seq(eng, ops, depth):
                    i = 0
                    while i < len(ops):
                        op = ops[i]
                        if len(op.cond) <= depth:
                            emit_op(eng, op)
                            i += 1
                            continue
                        key = op.cond[depth]
                        j = i
                        while j < len(ops) and len(ops[j].cond) > depth and ops[j].cond[depth] == key:
                            j += 1
                        grp = ops[i:j]
                        ninc = sum(1 for o in grp if o.need_inc)
                        r = self.regs[(engname, key[1])]
                        with eng.If_ne(r, 0):
                            emit_seq(eng, grp, depth + 1)
                        if ninc > 0:
                            with eng.Else():
                                self.dummy[engname](eng).then_inc(sems[engname], ninc)
                        i = j

                def body(eng):
                    emit_seq(eng, self.by_eng[engname], 0)
                return body

            if self.by_eng["pe"]:
                block.tensor(run("pe"))
            if self.by_eng["act"]:
                block.scalar(run("act"))
            if self.by_eng["dve"]:
                block.vector(run("dve"))
            if self.by_eng["pool"]:
                block.gpsimd(run("pool"))
            if self.by_eng["sp"]:
                block.sync(run("sp"))

    def stats(self):
        out = {}
        for e in self.ENGS:
            ops = self.by_eng[e]
            out[e] = (len(ops), sum(len(o.waits) for o in ops), sum(1 for o in ops if o.need_inc))
        return out


def _isap(x):
    return hasattr(x, "ap") and hasattr(x, "tensor")


class K:
    def __init__(self, S):
        self.S = S
        self._dk = 0

    def mm(self, out, lhsT, rhs, start=True, stop=True):
        return self.S.add("pe", lambda e: e.matmul(out, lhsT, rhs, start=start, stop=stop),
                          reads=[lhsT, rhs], writes=[out])

    def tr(self, out, in_, ident):
        return self.S.add("pe", lambda e: e.transpose(out, in_, ident), reads=[in_, ident], writes=[out])

    def act(self, out, in_, func, bias=0.0, scale=1.0, accum_out=None):
        reads = [in_] + [x for x in (bias, scale) if _isap(x)]
        writes = [out] + ([accum_out] if accum_out is not None else [])
        if accum_out is not None:
            fn = lambda e: e.activation(out, in_, func, bias=bias, scale=scale, accum_out=accum_out)
        else:
            fn = lambda e: e.activation(out, in_, func, bias=bias, scale=scale)
        return self.S.add("act", fn, reads=reads, writes=writes)

    def tt(self, out, in0, in1, op, eng="dve"):
        return self.S.add(eng, lambda e: e.tensor_tensor(out, in0, in1, op), reads=[in0, in1], writes=[out])

    def ts(self, out, in0, s1, s2, op0, op1=None, eng="dve", accum_out=None):
        reads = [in0] + [x for x in (s1, s2) if _isap(x)]
        writes = [out] + ([accum_out] if accum_out is not None else [])
        if op1 is None:
            fn = lambda e: e.tensor_single_scalar(out, in0, s1, op0)
        elif accum_out is not None:
            fn = lambda e: e.tensor_scalar(out, in0, s1, s2, op0, op1, accum_out=accum_out)
        else:
            fn = lambda e: e.tensor_scalar(out, in0, s1, s2, op0, op1)
        return self.S.add(eng, fn, reads=reads, writes=writes)

    def stt(self, out, in0, scalar, in1, op0, op1, eng="dve"):
        reads = [in0, in1] + ([scalar] if _isap(scalar) else [])
        return self.S.add(eng, lambda e: e.scalar_tensor_tensor(out, in0, scalar, in1, op0, op1),
                          reads=reads, writes=[out])

    def scan(self, out, d0, d1, initial, op0=ALU.mult, op1=ALU.add):
        reads = [d0, d1] + ([initial] if _isap(initial) else [])
        return self.S.add("dve", lambda e: e.tensor_tensor_scan(out, d0, d1, initial, op0, op1),
                          reads=reads, writes=[out])

    def copy(self, out, in_, eng="dve"):
        return self.S.add(eng, lambda e: e.tensor_copy(out, in_), reads=[in_], writes=[out])

    def memset(self, out, val, eng="dve"):
        return self.S.add(eng, lambda e: e.memset(out, val), reads=[], writes=[out])

    def recip(self, out, in_):
        return self.S.add("dve", lambda e: e.reciprocal(out, in_), reads=[in_], writes=[out])

    def reduce(self, out, in_, op, axis=AX.X):
        return self.S.add("dve", lambda e: e.tensor_reduce(out, in_, axis, op), reads=[in_], writes=[out])

    def dma(self, out, in_, q="pool", key=None):
        if key is None:
            self._dk += 1
            key = "u%d" % self._dk
        return self.S.add(q, lambda e: e.dma_start(out=out, in_=in_), reads=[in_], writes=[out], dma_key=key)


from concourse.bass_utils import run_bass_kernel_spmd

KB = 1024
NT = 1024
D = 2048
KC = 16
EPS = 1e-6
A0, A1, RING, TT, CC = 0, 32 * KB, 96 * KB, 160 * KB, 196 * KB
ARENA_BYTES = 207 * KB
NSLOT = 8

W_SPECS = [
    ("norm1_g", [2048]), ("w_in", [2048, 7168]), ("conv_w", [4, 1024]), ("conv_b", [1024]),
    ("w_rg_a", [16, 64, 64]), ("b_rg_a", [1024]), ("w_rg_x", [16, 64, 64]), ("b_rg_x", [1024]),
    ("lru_lambda", [1024]), ("w_pool", [4, 256, 256]), ("pool_scale", [1024]),
    ("w_branch_a", [1024, 2048]), ("w_branch_b", [1024, 2048]), ("w_out", [2048, 2048]),
    ("norm2_g", [2048]), ("w_router_group", [2048, 4]), ("b_router_group", [4]),
    ("w_router_expert", [2048, 16]), ("b_router_expert", [16]),
    ("w_e_gate", [16, 2048, 512]), ("w_e_up", [16, 2048, 512]), ("w_e_down", [16, 512, 2048]),
    ("norm_ple_g", [2048]), ("w_ple_gate", [2048, 2048]), ("w_ple_proj", [256, 2048]),
    ("final_norm_g", [2048]),
]


class _Stop(Exception):
    pass


def pipeline(n, stages):
    for it in range(n + len(stages) - 1):
        for si in range(len(stages) - 1, -1, -1):
            t = it - si
            if 0 <= t < n:
                stages[si](t)


def build_program(debug=(), stop_after=None):
    nc = bass.Bass("TRN2", target_bir_lowering=False)
    dr = {}
    for nm, shp in [("xm", [NT, D]), ("xp", [NT, D]), ("pm", [NT, 256]), ("flag", [128, 1]), ("invcnt", [128, 64])] + W_SPECS:
        dr[nm] = nc.dram_tensor(nm, shp, F32, kind="ExternalInput").ap()
    out = nc.dram_tensor("out", [NT, D], F32, kind="ExternalOutput").ap()
    x1s = nc.dram_tensor("x1_scratch", [NT, D], F32, kind="Internal").ap()
    dbg_out = {}
    es = ExitStack()
    with es:
        A = es.enter_context(nc.sbuf_tensor("arena", [128, ARENA_BYTES // 2], BF16))
        PS = es.enter_context(nc.psum_tensor("ps", [128, 8, 512], F32))
        S = Sched(nc)
        k = K(S)

        def view(off, shape, dt=BF16):
            n = 1
            for s in shape[1:]:
                n *= s
            e0 = off // 2
            ne = n * (2 if dt in (F32, I32) else 1)
            assert off % 4 == 0 and off + ne * 2 <= ARENA_BYTES, (off, shape)
            v = A[:, e0:e0 + ne]
            if dt in (F32, I32):
                v = v.bitcast(dt)
            if len(shape) == 3:
                v = v.rearrange("p (a b) -> p a b", a=shape[1])
            elif len(shape) == 4:
                v = v.rearrange("p (a b c) -> p a b c", a=shape[1], b=shape[2])
            if shape[0] < 128:
                v = v[0:shape[0]]
            return v

        bank_ctr = [0]

        held = set()

        def nb():
            assert len(held) < 7, "all PSUM banks held"
            while True:
                b = bank_ctr[0] % 7
                bank_ctr[0] += 1
                if b not in held:
                    return b

        def psb(b):
            return PS[:, b, :].bitcast(BF16)

        slot_ctr = [0]

        ring_addrs = [RING + i * 8 * KB for i in range(NSLOT)]

        def wload(src, shape):
            s = slot_ctr[0] % len(ring_addrs)
            slot_ctr[0] += 1
            v = view(ring_addrs[s], shape, BF16)
            k.dma(v, src, q="pool", key="ring%d" % s)
            return v

        def dump(name, v):
            if name in debug:
                t = nc.dram_tensor("dbg_" + name, list(v.shape), v.dtype, kind="ExternalOutput").ap()
                dbg_out[name] = k.dma(t, v, q="sp")
            if stop_after == name:
                raise _Stop()

        stores = []
        try:
            ident_bf = view(CC + 0, [128, 128], BF16)
            ident_f = view(CC + 256, [128, 128], F32)
            chv = view(CC + 768, [128, 9, 8], F32)
            c1h = view(CC + 1056, [128, 8], F32)
            c1f = view(CC + 1088, [128, 8], F32)
            stg = view(CC + 1120, [72, 128], F32)
            wr = view(CC + 1632, [128, 16, 20], F32)
            rb = view(CC + 2912, [128, 20], F32)
            invc = view(CC + 2992, [128, 64], F32)
            flg = view(CC + 3248, [128, 1], F32)
            bh = view(CC + 3264, [128, 2, 8], F32)
            ss = view(CC + 3328, [128, 16], F32)
            tmpv = view(CC + 3392, [128, 16], F32)
            rstd = view(CC + 3456, [128, 16], F32)
            stt_ = view(CC + 3520, [128, 8], F32)
            tmp16 = view(CC + 3552, [128, 16], F32)
            lg = view(CC + 3616, [128, 8, 20], F32)
            comb = view(CC + 4256, [128, 8, 16], F32)
            rsc = [view(CC + 4768 + i * 128, [128, 8, 4], F32) for i in range(12)]
            rs1 = [view(CC + 6304 + i * 32, [128, 8], F32) for i in range(8)]
            prod = view(CC + 6560, [128, 8, 4, 4], F32)

            k.memset(ident_f, 0.0)
            S.add("pool", lambda e: e.affine_select(out=ident_f, in_=ident_f, pattern=[[-1, 128]],
                                                    compare_op=ALU.not_equal, fill=1.0, base=0, channel_multiplier=1),
                  reads=[ident_f], writes=[ident_f])
            k.copy(ident_bf, ident_f)
            vecs = [dr["conv_w"][0], dr["conv_w"][1], dr["conv_w"][2], dr["conv_w"][3], dr["conv_b"],
                    dr["b_rg_a"], dr["b_rg_x"], dr["lru_lambda"], dr["pool_scale"]]
            for vi, vsrc in enumerate(vecs):
                k.dma(stg[vi * 8:(vi + 1) * 8, :], vsrc.rearrange("(c p) -> c p", p=128), q="sp")
            b0 = nb()
            k.tr(PS[:, b0, 0:72], stg, ident_f[0:72, 0:72])
            k.copy(chv.rearrange("p v c -> p (v c)"), PS[:, b0, 0:72])
            k.dma(flg, dr["flag"], q="sp")
            k.dma(invc, dr["invcnt"], q="sp")
            k.act(c1f, chv[:, 7, :], AF.Exp, scale=-1.0)
            k.act(c1f, c1f, AF.Ln, bias=1.0)
            k.ts(c1h, c1f, -4.0, None, ALU.mult)
            k.ts(c1f, c1f, -8.0, None, ALU.mult)
            k.ts(bh.rearrange("p a b -> p (a b)"), chv[:, 5:7, :].rearrange("p a b -> p (a b)"), 0.5, None, ALU.mult)
            k.dma(wr[:, :, 0:4], dr["w_router_group"].rearrange("(kc p) n -> p kc n", p=128), q="sp")
            k.dma(wr[:, :, 4:20], dr["w_router_expert"].rearrange("(kc p) n -> p kc n", p=128), q="sp")
            k.dma(rb[:, 0:4], dr["b_router_group"].partition_broadcast(128), q="sp")
            k.dma(rb[:, 4:20], dr["b_router_expert"].partition_broadcast(128), q="sp")

            hTp = view(A0, [128, 16, NT], BF16)
            uT = hTp
            htT = hTp
            hpT = hTp
            hT = view(A1, [128, 16, NT], BF16)
            y_a = view(A1 + 32 * KB, [128, 8, NT], BF16)
            y_b = view(A1 + 48 * KB, [128, 8, NT], BF16)
            x1v = view(A1, [128, 8, D], F32)

            def rms_stats(src, col):
                return col

            xld = [view(A1 + 32 * KB + i * 8 * KB, [128, D], F32) for i in range(4)]
            gbc = view(TT + 16 * KB, [128, D], F32)
            xnbs = [view(TT + 24 * KB, [128, D], BF16), view(TT + 28 * KB, [128, D], BF16)]
            k.dma(gbc, dr["norm1_g"].partition_broadcast(128), q="sp")
            evc = [0]
            sqjunk = view(TT + 32 * KB, [128, D], BF16)
            n1banks = {}

            def n1_s0(t):
                src = dr["xp"] if t < 8 else dr["xm"]
                r0 = (t % 8) * 128
                xl = xld[t % 4]
                k.dma(xl, src[r0:r0 + 128, :], q="sp", key="xld%d" % (t % 4))
                k.act(sqjunk, xl, AF.Square, accum_out=ss[:, t:t + 1])

            def n1_s1(t):
                k.ts(tmpv[:, t:t + 1], ss[:, t:t + 1], 1.0 / D, EPS, ALU.mult, ALU.add)

            def n1_s2(t):
                k.act(tmpv[:, t:t + 1], tmpv[:, t:t + 1], AF.Sqrt)

            def n1_s3(t):
                k.recip(rstd[:, t:t + 1], tmpv[:, t:t + 1])
                k.stt(xnbs[t % 2], xld[t % 4], rstd[:, t:t + 1], gbc, ALU.mult, ALU.mult)

            def n1_s4(t):
                xnb = xnbs[t % 2]
                bl = []
                for h in range(2):
                    b = nb()
                    held.add(b)
                    bl.append(b)
                    pb = psb(b)
                    for j in range(8):
                        kc = h * 8 + j
                        k.tr(pb[:, j * 128:(j + 1) * 128], xnb[:, kc * 128:(kc + 1) * 128], ident_bf)
                n1banks[t] = bl

            def n1_s5(t):
                r0 = (t % 8) * 128
                dstT = hTp if t < 8 else hT
                for h, b in enumerate(n1banks[t]):
                    dst = dstT[:, h * 8:(h + 1) * 8, r0:r0 + 128]
                    srcp = psb(b).rearrange("p (a b) -> p a b", a=8)
                    if h == 0:
                        k.act(dst, srcp, AF.Identity)
                    else:
                        k.copy(dst, srcp)
                    held.discard(b)

            pipeline(16, [n1_s0, n1_s1, n1_s2, n1_s3, n1_s4, n1_s5])
            dump("hT", hT)
            dump("hTp", hTp)

            YB0 = A1 + 48 * KB
            Wbd = view(YB0 + 12 * KB, [128, 8, 2, 128], BF16)
            k.memset(Wbd.rearrange("p a b c -> p (a b c)"), 0.0)
            for gi, wnm in enumerate(["w_rg_a", "w_rg_x"]):
                wsrc = dr[wnm].rearrange("(c h) i j -> h i c j", h=2)
                for h in range(2):
                    k.dma(Wbd[h * 64:(h + 1) * 64, :, gi, h * 64:(h + 1) * 64], wsrc[h], q="pool")
            RS7 = ring_addrs.pop(7)
            xr2 = [view(TT + i * 4112, [128, 1027], F32) for i in range(2)]
            xc3 = [view(TT + 8224, [128, NT], F32), view(TT + 12320, [128, NT], F32), view(RS7, [128, NT], F32)]
            xcb2 = [view(RS7 + 4096 + i * 2048, [128, NT], BF16) for i in range(2)]
            Ab2 = [view(TT + 16416 + i * 4096, [128, NT], F32) for i in range(2)]
            Gb2 = [view(TT + 24608 + i * 4096, [128, NT], F32) for i in range(2)]
            Mb2 = [view(YB0 + i * 4096, [128, NT], F32) for i in range(2)]
            Yb = view(YB0 + 8 * KB, [128, NT], F32)
            halo_sv = view(CC + 11248, [128, 3], F32)
            w_in_v = dr["w_in"].rearrange("(kc p) c -> p kc c", p=128)
            chains = [(c, hf) for c in range(8) for hf in range(2)]
            wxs, wgs_ = {}, {}
            zbanks, ybanks = {}, {}

            def rn0(t):
                c, hf = chains[t]
                cc = c % 2
                if cc == 0 and hf == 0:
                    wxs[c // 2] = wload(w_in_v[:, :, c * 128:c * 128 + 256], [128, 16, 256])
                    wgs_[c // 2] = wload(w_in_v[:, :, 1024 + c * 128:1024 + c * 128 + 256], [128, 16, 256])
                wx = wxs[c // 2]
                hsrc = hTp if hf == 0 else hT
                bl = []
                for blk in range(2):
                    b = nb()
                    held.add(b)
                    bl.append(b)
                    for kc in range(KC):
                        k.mm(PS[:, b, :], wx[:, kc, cc * 128:(cc + 1) * 128], hsrc[:, kc, blk * 512:(blk + 1) * 512],
                             start=(kc == 0), stop=(kc == KC - 1))
                zbanks[t] = bl

            def rn1(t):
                xr = xr2[t % 2]
                for blk, b in enumerate(zbanks[t]):
                    k.act(xr[:, 3 + blk * 512:3 + (blk + 1) * 512], PS[:, b, :], AF.Identity)
                    held.discard(b)

            def rn2(t):
                c, hf = chains[t]
                cc = c % 2
                xr, xc, xcb = xr2[t % 2], xc3[t % 3], xcb2[t % 2]
                if hf == 0:
                    k.memset(xr[:, 0:3], 0.0)
                else:
                    k.copy(xr[:, 0:3], halo_sv)
                k.ts(xc, xr[:, 3:3 + NT], chv[:, 3, c:c + 1], chv[:, 4, c:c + 1], ALU.mult, ALU.add)
                for kk in range(3):
                    k.stt(xc, xr[:, kk:kk + NT], chv[:, kk, c:c + 1], xc, ALU.mult, ALU.add)
                if hf == 0:
                    k.copy(halo_sv, xr[:, NT:NT + 3])
                k.copy(xcb, xc)
                if hf == 1:
                    wg = wgs_[c // 2]
                    bl = []
                    for blk in range(2):
                        b = nb()
                        held.add(b)
                        bl.append(b)
                        for kc in range(KC):
                            k.mm(PS[:, b, :], wg[:, kc, cc * 128:(cc + 1) * 128], hT[:, kc, blk * 512:(blk + 1) * 512],
                                 start=(kc == 0), stop=(kc == KC - 1))
                    ybanks[t] = bl

            def rn3(t):
                c, hf = chains[t]
                xcb, Ab, Gb, Mb = xcb2[t % 2], Ab2[t % 2], Gb2[t % 2], Mb2[t % 2]
                for gi, buf in enumerate([Ab, Gb]):
                    for blk in range(2):
                        b = nb()
                        k.mm(PS[:, b, :], Wbd[:, c, gi, :], xcb[:, blk * 512:(blk + 1) * 512])
                        k.act(buf[:, blk * 512:(blk + 1) * 512], PS[:, b, :], AF.Tanh,
                              bias=bh[:, gi, c:c + 1], scale=0.5)
                k.act(Mb, Ab, AF.Exp, bias=c1f[:, c:c + 1], scale=c1f[:, c:c + 1])
                k.act(Ab, Ab, AF.Exp, bias=c1h[:, c:c + 1], scale=c1h[:, c:c + 1])
                k.act(Mb, Mb, AF.Sqrt, bias=1.0, scale=-1.0)
                if hf == 1:
                    for blk, b in enumerate(ybanks[t]):
                        k.act(Yb[:, blk * 512:(blk + 1) * 512], PS[:, b, :], AF.Gelu_apprx_tanh)
                        held.discard(b)

            def rn4(t):
                c, hf = chains[t]
                xc, Ab, Gb, Mb = xc3[t % 3], Ab2[t % 2], Gb2[t % 2], Mb2[t % 2]
                k.stt(Gb, Gb, 1.0, Mb, ALU.add, ALU.mult)
                k.stt(Gb, Gb, 0.5, xc, ALU.mult, ALU.mult)
                if hf == 0:
                    k.scan(Mb, Ab, Gb, 0.0)
                    k.tt(stt_[:, c:c + 1], Mb[:, NT - 1:NT], flg, ALU.mult)
                else:
                    k.scan(Mb, Ab, Gb, stt_[:, c:c + 1])
                    k.tt(y_a[:, c, :], Yb, Mb, ALU.mult)

            pipeline(16, [rn0, rn1, rn2, rn3, rn4])
            ring_addrs.insert(7, RS7)
            dump("y_a", y_a)

            P0 = view(TT + 0, [128, 16 + NT], F32)
            P1 = view(TT + 4160, [128, 16 + NT], F32)
            P2 = view(TT + 8320, [128, 16 + NT], F32)
            dbf = view(TT + 12480, [128, 2, NT], BF16)
            wpl = view(TT + 16576, [128, 4, 2, 256], BF16)
            k.dma(wpl, dr["w_pool"].rearrange("g (kc p) d -> p g kc d", p=128), q="pool")
            wp = None
            L = 16 + NT
            for pc in range(8):
                cc = pc % 2
                g = pc // 2
                w = 2 << g
                if cc == 0:
                    wp = wload(w_in_v[:, :, 2048 + pc * 128:2048 + pc * 128 + 256], [128, 16, 256])
                b = nb()
                for kc in range(KC):
                    k.mm(PS[:, b, 0:16], wp[:, kc, cc * 128:(cc + 1) * 128], hTp[:, kc, NT - 16:NT],
                         start=(kc == 0), stop=(kc == KC - 1))
                k.act(P0[:, 0:16], PS[:, b, 0:16], AF.Identity)
                for blk in range(2):
                    b = nb()
                    for kc in range(KC):
                        k.mm(PS[:, b, :], wp[:, kc, cc * 128:(cc + 1) * 128], hT[:, kc, blk * 512:(blk + 1) * 512],
                             start=(kc == 0), stop=(kc == KC - 1))
                    k.act(P0[:, 16 + blk * 512:16 + (blk + 1) * 512], PS[:, b, :], AF.Identity)
                cur, oth = P0, [P1, P2]
                sh = 1
                for st in range(g + 1):
                    dst = oth[st % 2]
                    lo = 2 * sh - 1
                    k.tt(dst[:, lo:L], cur[:, lo:L], cur[:, lo - sh:L - sh], ALU.add)
                    cur = dst
                    sh *= 2
                k.stt(dbf[:, cc, :], cur[:, 16:L], 1.0 / w, P0[:, 16:L], ALU.mult, ALU.subtract)
                k.tt(tmp16, cur[:, 16:32], invc[:, g * 16:(g + 1) * 16], ALU.mult)
                k.tt(dbf[:, cc, 0:16], tmp16, P0[:, 16:32], ALU.subtract)
                if cc == 1:
                    for oc in range(2):
                        for blk in range(2):
                            b = nb()
                            for kc2 in range(2):
                                k.mm(PS[:, b, :], wpl[:, g, kc2, oc * 128:(oc + 1) * 128], dbf[:, kc2, blk * 512:(blk + 1) * 512],
                                     start=(kc2 == 0), stop=(kc2 == 1))
                            k.act(y_b[:, g * 2 + oc, blk * 512:(blk + 1) * 512], PS[:, b, :], AF.Identity,
                                  scale=chv[:, 8, g * 2 + oc:g * 2 + oc + 1])
            dump("y_b", y_b)

            sA = [view(TT + i * 2 * KB, [128, 512], F32) for i in range(2)]
            sB = [view(TT + 4 * KB + i * 2 * KB, [128, 512], F32) for i in range(2)]
            t1 = [view(TT + 8 * KB + i * 2 * KB, [128, 512], F32) for i in range(2)]
            wa_v = dr["w_branch_a"].rearrange("(kc p) c -> p kc c", p=128)
            wb_v = dr["w_branch_b"].rearrange("(kc p) c -> p kc c", p=128)
            ga = gb = wA = wB = None
            it = 0
            for oc in range(16):
                if oc % 4 == 0:
                    wA = wload(wa_v[:, :, oc * 128:oc * 128 + 512], [128, 8, 512])
                    wB = wload(wb_v[:, :, oc * 128:oc * 128 + 512], [128, 8, 512])
                if oc % 2 == 0:
                    ga = wload(w_in_v[:, :, 3072 + oc * 128:3072 + oc * 128 + 256], [128, 16, 256])
                    gb = wload(w_in_v[:, :, 5120 + oc * 128:5120 + oc * 128 + 256], [128, 16, 256])
                c2 = oc % 2
                c4 = oc % 4
                for blk in range(2):
                    bs = slice(blk * 512, (blk + 1) * 512)
                    i2 = it % 2
                    it += 1
                    b1 = nb()
                    for kc in range(KC):
                        k.mm(PS[:, b1, :], ga[:, kc, c2 * 128:(c2 + 1) * 128], hT[:, kc, bs], start=(kc == 0), stop=(kc == KC - 1))
                    k.act(sA[i2], PS[:, b1, :], AF.Sigmoid)
                    b2 = nb()
                    for kc in range(8):
                        k.mm(PS[:, b2, :], wA[:, kc, c4 * 128:(c4 + 1) * 128], y_a[:, kc, bs], start=(kc == 0), stop=(kc == 7))
                    k.tt(sA[i2], sA[i2], PS[:, b2, :], ALU.mult)
                    b3 = nb()
                    for kc in range(KC):
                        k.mm(PS[:, b3, :], gb[:, kc, c2 * 128:(c2 + 1) * 128], hT[:, kc, bs], start=(kc == 0), stop=(kc == KC - 1))
                    k.act(sB[i2], PS[:, b3, :], AF.Sigmoid)
                    b4 = nb()
                    for kc in range(8):
                        k.mm(PS[:, b4, :], wB[:, kc, c4 * 128:(c4 + 1) * 128], y_b[:, kc, bs], start=(kc == 0), stop=(kc == 7))
                    k.tt(sB[i2], sB[i2], PS[:, b4, :], ALU.mult)
                    k.tt(uT[:, oc, bs], sA[i2], sB[i2], ALU.add)
            dump("uT", uT)

            xrl1 = view(TT + 28 * KB, [128, D], F32)
            wo_v = dr["w_out"].rearrange("(kc p) c -> p kc c", p=128)
            wo_all = [[wload(wo_v[:, h * 8:(h + 1) * 8, db * 512:(db + 1) * 512], [128, 8, 512]) for h in range(2)]
                      for db in range(4)]
            gbc2 = view(TT + 0, [128, D], F32)
            xn32s = [view(TT + 8 * KB, [128, D], F32)] * 2
            ht32T = view(TT + 16 * KB, [128, 16, 128], F32)
            k.dma(gbc2, dr["norm2_g"].partition_broadcast(128), q="sp")

            sqjunk2 = view(TT + 24 * KB, [128, D], BF16)
            rtbanks, rtb2 = {}, {}

            def rt_s0(i):
                k.act(sqjunk2, x1v[:, i, :], AF.Square, accum_out=ss[:, i:i + 1])

            def rt_s1(i):
                k.ts(tmpv[:, i:i + 1], ss[:, i:i + 1], 1.0 / D, EPS, ALU.mult, ALU.add)

            def rt_s2(i):
                k.act(tmpv[:, i:i + 1], tmpv[:, i:i + 1], AF.Sqrt)

            def rt_s3(i):
                k.recip(rstd[:, i:i + 1], tmpv[:, i:i + 1])
                k.stt(xn32s[i % 2], x1v[:, i, :], rstd[:, i:i + 1], gbc2, ALU.mult, ALU.mult)

            def rt_s4(i):
                xn32 = xn32s[i % 2]
                bl = []
                for q in range(4):
                    b = nb()
                    held.add(b)
                    bl.append(b)
                    for j in range(4):
                        kc = q * 4 + j
                        k.tr(PS[:, b, j * 128:(j + 1) * 128], xn32[:, kc * 128:(kc + 1) * 128], ident_f)
                rtbanks[i] = bl

            def rt_s5(i):
                for q, b in enumerate(rtbanks[i]):
                    srcp = PS[:, b, :].rearrange("p (a b) -> p a b", a=4)
                    if q % 2 == 0:
                        k.act(ht32T[:, q * 4:(q + 1) * 4, :], srcp, AF.Identity)
                    else:
                        k.copy(ht32T[:, q * 4:(q + 1) * 4, :], srcp)
                    held.discard(b)
                b = nb()
                held.add(b)
                rtb2[i] = b
                for kc in range(KC):
                    k.mm(PS[:, b, 0:20], ht32T[:, kc, :], wr[:, kc, :], start=(kc == 0), stop=(kc == KC - 1))

            def rt_s6(i):
                b = rtb2[i]
                k.tt(lg[:, i, :], PS[:, b, 0:20], rb, ALU.add)
                held.discard(b)


            def wout_tile(i):
                k.dma(xrl1, dr["xm"][i * 128:(i + 1) * 128, :], q="sp", key="xrl1")
                for db in range(4):
                    ds_ = slice(db * 512, (db + 1) * 512)
                    b = nb()
                    for kc in range(KC):
                        k.mm(PS[:, b, :], uT[:, kc, i * 128:(i + 1) * 128], wo_all[db][kc // 8][:, kc % 8, :],
                             start=(kc == 0), stop=(kc == KC - 1))
                    k.tt(x1v[:, i, ds_], xrl1[:, ds_], PS[:, b, :], ALU.add)

            pipeline(8, [wout_tile, rt_s0, rt_s1, rt_s2, rt_s3, rt_s4, rt_s5, rt_s6])
            x1s_w = []
            for i in range(8):
                x1s_w.append(k.dma(x1s[i * 128:(i + 1) * 128, :], x1v[:, i, :], q="sp"))
            dump("x1", x1v)

            dmyA = view(CC + 7072, [128, 1], F32)
            dmyD = view(CC + 7088, [128, 1], F32)
            k.memset(dmyA, 0.0)
            k.memset(dmyD, 0.0)
            S.dummy["pe"] = lambda e: e.matmul(PS[:, 7, 0:2], ident_bf, ident_bf[:, 0:2], start=True, stop=True)
            S.dummy["act"] = lambda e: e.activation(dmyA, dmyA, AF.Identity)
            S.dummy["dve"] = lambda e: e.memset(dmyD, 0.0)
            Lf = view(CC + 7104, [128, 128], F32)
            Ltri = view(CC + 7616, [128, 128], BF16)
            ones_bf = view(CC + 7872, [128, 128], BF16)
            siota = view(CC + 8128, [128, 8], F32)
            jv = view(CC + 8160, [128, 8], F32)
            oh_bf = view(CC + 8192, [128, 32], BF16)
            tot_s = view(CC + 8256, [128, 8, 4], F32)
            wit_s = view(CC + 8384, [128, 8, 4], F32)
            tpre = view(CC + 8512, [128, 8, 4], F32)
            ngv = view(CC + 8640, [128, 4], F32)
            offv = view(CC + 8656, [128, 4], F32)
            endv = view(CC + 8672, [128, 4], F32)
            posv = view(CC + 8688, [128, 8], F32)
            fl_a = view(CC + 8720, [128, 4, 8], F32)
            fl_b = view(CC + 8848, [128, 4, 8], F32)
            fl_i = view(CC + 8976, [128, 4, 8], I32)
            cmb_hl = view(CC + 9104, [128, 8, 32], BF16)
            cmb_t = view(CC + 9616, [128, 8, 16], F32)
            comb_s = view(CC + 10128, [128, 8, 16], F32)
            iint = view(CC + 10640, [128, 16], I32)
            umask = view(CC + 10832, [128, 4, 8], F32)
            flU = view(CC + 10960, [128, 4], F32)
            flU_i = view(CC + 10976, [128, 4], I32)
            LIKELY = [(0, 3), (1, 5), (3, 7), (5, 8)]
            c32 = view(CC + 10704, [128, 32], F32)
            k.memset(Lf, 1.0)
            S.add("pool", lambda e: e.affine_select(out=Lf, in_=Lf, pattern=[[1, 128]], compare_op=ALU.is_gt,
                                                    fill=0.0, base=0, channel_multiplier=-1), reads=[Lf], writes=[Lf])
            k.copy(Ltri, Lf)
            k.memset(Lf, 1.0)
            k.memset(ones_bf, 1.0)
            S.add("pool", lambda e: e.iota(iint[:, 0:8], pattern=[[128, 8]], base=0, channel_multiplier=1), writes=[iint[:, 0:8]])
            S.add("pool", lambda e: e.iota(iint[:, 8:16], pattern=[[128, 8]], base=0, channel_multiplier=0), writes=[iint[:, 8:16]])
            k.copy(siota, iint[:, 0:8])
            k.copy(jv, iint[:, 8:16])

            dump("lg", lg)
            lgG = lg[:, :, 0:4]
            lgE4 = lg[:, :, 4:20].rearrange("p t (g j) -> p t j g", g=4)
            mx, sg_, gw, m1, m2, se, gw2 = rs1[0], rs1[1], rs1[2], rs1[3], rs1[4], rs1[5], rs1[6]
            oh, eg, lsel, msk, le2, sel, ee, wt4 = rsc[0], rsc[1], rsc[2], rsc[3], rsc[4], rsc[5], rsc[6], rsc[7]
            basev, posg = rsc[8], rsc[9]

            def bc3(v):
                return v.unsqueeze(2).to_broadcast([128, 8, 4])
            k.reduce(mx, lgG, ALU.max)
            k.tt(oh, lgG, bc3(mx), ALU.is_equal)
            k.tt(eg, lgG, bc3(mx), ALU.subtract)
            k.act(eg, eg, AF.Exp)
            k.reduce(sg_, eg, ALU.add)
            k.recip(gw, sg_)
            k.tt(prod, lgE4, oh.unsqueeze(2).to_broadcast([128, 8, 4, 4]), ALU.mult)
            k.reduce(lsel, prod, ALU.add)
            k.reduce(m1, lsel, ALU.max)
            k.tt(msk, lsel, bc3(m1), ALU.is_equal)
            k.stt(le2, msk, -1e30, lsel, ALU.mult, ALU.add)
            k.reduce(m2, le2, ALU.max)
            k.tt(sel, lsel, bc3(m2), ALU.is_ge)
            k.tt(ee, lsel, bc3(m1), ALU.subtract)
            k.act(ee, ee, AF.Exp)
            k.tt(ee, ee, sel, ALU.mult)
            k.reduce(se, ee, ALU.add)
            k.recip(gw2, se)
            k.tt(gw2, gw2, gw, ALU.mult)
            k.tt(wt4, ee, bc3(gw2), ALU.mult)
            comb4 = comb.rearrange("p t (g j) -> p t g j", g=4)
            k.tt(comb4, oh.unsqueeze(3).to_broadcast([128, 8, 4, 4]), wt4.unsqueeze(2).to_broadcast([128, 8, 4, 4]), ALU.mult)
            dump("comb", comb)

            k.copy(oh_bf, oh.rearrange("p t g -> p (t g)"))
            b = nb()
            k.mm(PS[:, b, 0:32], ones_bf, oh_bf)
            k.copy(tot_s.rearrange("p t g -> p (t g)"), PS[:, b, 0:32])
            b = nb()
            k.mm(PS[:, b, 0:32], Ltri, oh_bf)
            k.copy(wit_s.rearrange("p t g -> p (t g)"), PS[:, b, 0:32])
            k.memset(tpre[:, 0, :], 0.0)
            for i in range(1, 8):
                k.tt(tpre[:, i, :], tpre[:, i - 1, :], tot_s[:, i - 1, :], ALU.add)
            k.tt(ngv, tpre[:, 7, :], tot_s[:, 7, :], ALU.add)
            k.memset(offv[:, 0:1], 0.0)
            for g in range(1, 4):
                k.tt(offv[:, g:g + 1], offv[:, g - 1:g], ngv[:, g - 1:g], ALU.add)
            k.tt(endv, offv, ngv, ALU.add)
            k.tt(basev, tpre, offv.unsqueeze(1).to_broadcast([128, 8, 4]), ALU.add)
            k.tt(basev, basev, wit_s, ALU.add)
            k.tt(posg, basev, oh, ALU.mult)
            k.reduce(posv, posg, ALU.add)
            offb = offv.unsqueeze(2).to_broadcast([128, 4, 8])
            endb = endv.unsqueeze(2).to_broadcast([128, 4, 8])
            jvb = jv.unsqueeze(1).to_broadcast([128, 4, 8])
            k.tt(fl_a, offb, jvb, ALU.subtract)
            k.ts(fl_a, fl_a, 128.0, None, ALU.is_lt)
            k.tt(fl_b, endb, jvb, ALU.subtract)
            k.ts(fl_b, fl_b, 0.0, None, ALU.is_gt)
            k.tt(fl_a, fl_a, fl_b, ALU.mult)
            k.copy(fl_i, fl_a)
            k.memset(umask.rearrange("p g j -> p (g j)"), 1.0)
            for g in range(4):
                lo_, hi_ = LIKELY[g]
                k.memset(umask[:, g, lo_:hi_], 0.0)
            k.tt(umask, umask, fl_a, ALU.mult)
            k.reduce(flU, umask, ALU.max)
            k.copy(flU_i, flU)
            dump("posv", posv)
            dump("fl_a", fl_a)

            iota_i = view(A0, [128, NT], I32)
            iota_s = view(A0 + 4 * KB, [128, NT], F32)
            Pm = view(TT + 0, [128, 8, NT], BF16)
            S.add("pool", lambda e: e.iota(iota_i, pattern=[[1, NT]], base=0, channel_multiplier=0), writes=[iota_i])
            k.copy(iota_s, iota_i)
            for i in range(8):
                k.ts(Pm[:, i, :], iota_s, posv[:, i:i + 1], None, ALU.is_equal)
            k.copy(cmb_hl[:, :, 0:16], comb)
            k.tt(cmb_t, comb, cmb_hl[:, :, 0:16], ALU.subtract)
            k.copy(cmb_hl[:, :, 16:32], cmb_t)
            for j in range(8):
                b = nb()
                for i in range(8):
                    k.mm(PS[:, b, 0:32], Pm[:, i, j * 128:(j + 1) * 128], cmb_hl[:, i, :], start=(i == 0), stop=(i == 7))
                k.copy(c32, PS[:, b, 0:32])
                k.tt(comb_s[:, j, :], c32[:, 0:16], c32[:, 16:32], ALU.add)
            dump("comb_s", comb_s)

            ht_tok = view(TT + 16 * KB, [128, 8, 1024], BF16)
            gbh = view(TT + 32 * KB, [128, 1024], F32)
            htS = hTp
            ev = 0
            for hh in range(2):
                fsl = slice(hh * 1024, (hh + 1) * 1024)
                k.dma(gbh, dr["norm2_g"][fsl].partition_broadcast(128), q="sp", key="gbh")
                for i in range(8):
                    k.stt(ht_tok[:, i, :], x1v[:, i, fsl], rstd[:, i:i + 1], gbh, ALU.mult, ALU.mult)
                for kc8 in range(8):
                    kc = hh * 8 + kc8
                    for sb in range(2):
                        b = nb()
                        for i in range(8):
                            k.mm(PS[:, b, :], ht_tok[:, i, kc8 * 128:(kc8 + 1) * 128], Pm[:, i, sb * 512:(sb + 1) * 512],
                                 start=(i == 0), stop=(i == 7))
                        dst = htS[:, kc, sb * 512:(sb + 1) * 512]
                        if ev % 2 == 0:
                            k.act(dst, PS[:, b, :], AF.Identity)
                        else:
                            k.copy(dst, PS[:, b, :])
                        ev += 1
            dump("htS", htS)

            ysum = x1v
            for j in range(8):
                k.memset(ysum[:, j, :], 0.0)

            slb = [view(TT + i * 2 * KB, [128, 512], F32) for i in range(2)]
            atok = [view(TT + 4 * KB + i * KB, [128, 512], BF16) for i in range(2)]
            aT_all = [view(TT + 6 * KB, [128, 8, 4, 128], BF16)] * 2
            ring_addrs.extend([TT + 16 * KB, TT + 24 * KB])
            bi = 0
            for g in range(4):
                for en in ("pe", "act", "dve"):
                    for j in range(8):
                        S.flagload(en, "j%d" % j, fl_i[0:1, g, j:j + 1])
                    S.flagload(en, "U", flU_i[0:1, g:g + 1])
                lo_, hi_ = LIKELY[g]
                jorder = list(range(lo_, hi_)) + [j for j in range(8) if not (lo_ <= j < hi_)]
                for e4 in range(4):
                    e = g * 4 + e4
                    wgv = dr["w_e_gate"][e].rearrange("(kc p) f -> p kc f", p=128)
                    wuv = dr["w_e_up"][e].rearrange("(kc p) f -> p kc f", p=128)
                    wdv = dr["w_e_down"][e].rearrange("(fc p) d -> p fc d", p=128)
                    wgs = [wload(wgv[:, h * 8:(h + 1) * 8, :], [128, 8, 512]) for h in range(2)]
                    wus = [wload(wuv[:, h * 8:(h + 1) * 8, :], [128, 8, 512]) for h in range(2)]
                    wds = [wload(wdv[:, :, h * 1024:(h + 1) * 1024], [128, 4, 1024]) for h in range(2)]
                    aTe = aT_all[e % 2]
                    in_outer = False
                    for j in jorder:
                        unlikely = not (lo_ <= j < hi_)
                        if unlikely and not in_outer:
                            S.cond_begin(("Ua", g, e4), "U")
                            in_outer = True
                        S.cond_begin((g, e4, j, "a"), "j%d" % j)
                        js = slice(j * 128, (j + 1) * 128)
                        bg = nb()
                        for kc in range(KC):
                            k.mm(PS[:, bg, :], htS[:, kc, js], wgs[kc // 8][:, kc % 8, :], start=(kc == 0), stop=(kc == KC - 1))
                        bu = nb()
                        for kc in range(KC):
                            k.mm(PS[:, bu, :], htS[:, kc, js], wus[kc // 8][:, kc % 8, :], start=(kc == 0), stop=(kc == KC - 1))
                        sl = slb[bi % 2]
                        at = atok[bi % 2]
                        bi += 1
                        k.act(sl, PS[:, bg, :], AF.Silu)
                        k.tt(at, sl, PS[:, bu, :], ALU.mult)
                        bt = nb()
                        pbt = psb(bt)
                        for fc in range(4):
                            k.tr(pbt[:, fc * 128:(fc + 1) * 128], at[:, fc * 128:(fc + 1) * 128], ident_bf)
                        k.act(aTe[:, j, :, :], pbt[:, 0:512].rearrange("p (a b) -> p a b", a=4), AF.Identity)
                        S.cond_end()
                    if in_outer:
                        S.cond_end()
                    in_outer = False
                    for j in jorder:
                        unlikely = not (lo_ <= j < hi_)
                        if unlikely and not in_outer:
                            S.cond_begin(("Ub", g, e4), "U")
                            in_outer = True
                        S.cond_begin((g, e4, j, "b"), "j%d" % j)
                        for db in range(4):
                            b = nb()
                            for fc in range(4):
                                k.mm(PS[:, b, :], aTe[:, j, fc, :], wds[db // 2][:, fc, (db % 2) * 512:(db % 2 + 1) * 512],
                                     start=(fc == 0), stop=(fc == 3))
                            ys = ysum[:, j, db * 512:(db + 1) * 512]
                            k.stt(ys, PS[:, b, :], comb_s[:, j, e:e + 1], ys, ALU.mult, ALU.add)
                        S.cond_end()
                    if in_outer:
                        S.cond_end()
            del ring_addrs[NSLOT:]
            dump("ysum", ysum)

            posrow = view(TT + 8 * KB, [128, NT], F32)
            dg = view(TT + 12 * KB, [128, 128], F32)
            PT = view(TT + 16 * KB, [128, 8, NT], BF16)
            yh = view(A0, [128, 8, 1024], BF16)
            yl = view(A0 + 16 * KB, [128, 8, 1024], BF16)
            xn32 = xn32s[0]
            ytmp = view(TT + 32 * KB, [128, 1024], F32)
            for hb in range(2):
                b = nb()
                for i4 in range(4):
                    i = hb * 4 + i4
                    k.ts(dg, ident_f, posv[:, i:i + 1], None, ALU.mult)
                    k.mm(PS[:, b, i4 * 128:(i4 + 1) * 128], Lf, dg)
                k.copy(posrow[:, hb * 512:(hb + 1) * 512], PS[:, b, :])
            for j in range(8):
                k.ts(PT[:, j, :], posrow, siota[:, j:j + 1], None, ALU.is_equal)
            cv = 0
            yhs = [yh, yl]
            for dh in range(2):
                dsl = slice(dh * 1024, (dh + 1) * 1024)
                yh = yhs[dh]
                for j in range(8):
                    if j % 2 == 0:
                        k.act(yh[:, j, :], ysum[:, j, dsl], AF.Copy)
                    else:
                        k.copy(yh[:, j, :], ysum[:, j, dsl])
                for i in range(8):
                    dst = x1v[:, i, dsl]
                    src = x1s[i * 128:(i + 1) * 128, dsl]
                    S.add("sp", (lambda e, dst=dst, src=src: e.dma_start(out=dst, in_=src)), reads=[], writes=[dst],
                          dma_key="x1r%d_%d" % (dh, i), extra_deps=[x1s_w[i]])
                for i in range(8):
                    for d2 in range(2):
                        b = nb()
                        n = 0
                        for j in range(8):
                            k.mm(PS[:, b, :], PT[:, j, i * 128:(i + 1) * 128], yh[:, j, d2 * 512:(d2 + 1) * 512],
                                 start=(j == 0), stop=(j == 7))
                        dcol = slice(dh * 1024 + d2 * 512, dh * 1024 + (d2 + 1) * 512)
                        k.tt(x1v[:, i, dcol], x1v[:, i, dcol], PS[:, b, :], ALU.add)
            dump("x2", x1v)

            gbc3 = view(TT + 0, [128, D], F32)
            xnb3s = [view(TT + 8 * KB + i * 4 * KB, [128, D], BF16) for i in range(2)]
            pT = view(TT + 16 * KB, [128, 2, NT], BF16)
            pl = [view(TT + 20 * KB + i * KB, [128, 256], F32) for i in range(2)]
            pbf = view(TT + 22 * KB, [128, 256], BF16)
            sg3 = [view(TT + 23 * KB + i * 2 * KB, [128, 512], F32) for i in range(2)]
            sqjunk3 = view(TT + 27 * KB, [128, D], BF16)
            ost = [view(TT + 8 * KB + i * 8 * KB, [128, D], F32) for i in range(2)]
            k.dma(gbc3, dr["norm_ple_g"].partition_broadcast(128), q="sp")
            for i in range(8):
                k.dma(pl[i % 2], dr["pm"][i * 128:(i + 1) * 128, :], q="sp", key="pl%d" % (i % 2))
                k.copy(pbf, pl[i % 2])
                b = nb()
                pb = psb(b)
                for j in range(2):
                    k.tr(pb[:, j * 128:(j + 1) * 128], pbf[:, j * 128:(j + 1) * 128], ident_bf)
                k.copy(pT[:, :, i * 128:(i + 1) * 128], pb[:, 0:256].rearrange("p (a b) -> p a b", a=2))
            plbanks = {}

            def pl_s0(i):
                k.act(sqjunk3, x1v[:, i, :], AF.Square, accum_out=ss[:, i:i + 1])

            def pl_s1(i):
                k.ts(tmpv[:, i:i + 1], ss[:, i:i + 1], 1.0 / D, EPS, ALU.mult, ALU.add)

            def pl_s2(i):
                k.act(tmpv[:, i:i + 1], tmpv[:, i:i + 1], AF.Sqrt)

            def pl_s3(i):
                k.recip(rstd[:, i:i + 1], tmpv[:, i:i + 1])
                k.stt(xnb3s[i % 2], x1v[:, i, :], rstd[:, i:i + 1], gbc3, ALU.mult, ALU.mult)

            def pl_s4(i):
                xnb3 = xnb3s[i % 2]
                bl = []
                for h in range(2):
                    b = nb()
                    held.add(b)
                    bl.append(b)
                    pb = psb(b)
                    for j in range(8):
                        kc = h * 8 + j
                        k.tr(pb[:, j * 128:(j + 1) * 128], xnb3[:, kc * 128:(kc + 1) * 128], ident_bf)
                plbanks[i] = bl

            def pl_s5(i):
                for h, b in enumerate(plbanks[i]):
                    dst = hpT[:, h * 8:(h + 1) * 8, i * 128:(i + 1) * 128]
                    srcp = psb(b).rearrange("p (a b) -> p a b", a=8)
                    if h == 0:
                        k.act(dst, srcp, AF.Identity)
                    else:
                        k.copy(dst, srcp)
                    held.discard(b)

            pipeline(8, [pl_s0, pl_s1, pl_s2, pl_s3, pl_s4, pl_s5])
            wpp_v = dr["w_ple_proj"].rearrange("(kc p) d -> p kc d", p=128)
            wpg_v = dr["w_ple_gate"].rearrange("(kc p) c -> p kc c", p=128)
            it = 0
            for db in range(4):
                ds_ = slice(db * 512, (db + 1) * 512)
                wpg = [wload(wpg_v[:, h * 8:(h + 1) * 8, ds_], [128, 8, 512]) for h in range(2)]
                wpp = wload(wpp_v[:, :, ds_], [128, 2, 512])
                for i in range(8):
                    ts_ = slice(i * 128, (i + 1) * 128)
                    b1 = nb()
                    for kc in range(KC):
                        k.mm(PS[:, b1, :], hpT[:, kc, ts_], wpg[kc // 8][:, kc % 8, :], start=(kc == 0), stop=(kc == KC - 1))
                    b2 = nb()
                    for kc2 in range(2):
                        k.mm(PS[:, b2, :], pT[:, kc2, ts_], wpp[:, kc2, :], start=(kc2 == 0), stop=(kc2 == 1))
                    sgt = sg3[it % 2]
                    it += 1
                    k.act(sgt, PS[:, b1, :], AF.Sigmoid)
                    k.tt(sgt, sgt, PS[:, b2, :], ALU.mult)
                    k.tt(x1v[:, i, ds_], x1v[:, i, ds_], sgt, ALU.add)
            k.dma(gbc3, dr["final_norm_g"].partition_broadcast(128), q="sp")
            def fn_s0(i):
                k.act(sqjunk3, x1v[:, i, :], AF.Square, accum_out=ss[:, 8 + i:9 + i])

            def fn_s1(i):
                k.ts(tmpv[:, 8 + i:9 + i], ss[:, 8 + i:9 + i], 1.0 / D, EPS, ALU.mult, ALU.add)

            def fn_s2(i):
                k.act(tmpv[:, 8 + i:9 + i], tmpv[:, 8 + i:9 + i], AF.Sqrt)

            def fn_s3(i):
                k.recip(rstd[:, 8 + i:9 + i], tmpv[:, 8 + i:9 + i])
                k.stt(ost[i % 2], x1v[:, i, :], rstd[:, 8 + i:9 + i], gbc3, ALU.mult, ALU.mult)

            def fn_s4(i):
                stores.append(k.dma(out[i * 128:(i + 1) * 128, :], ost[i % 2], q="sp", key="ost%d" % (i % 2)))

            pipeline(8, [fn_s0, fn_s1, fn_s2, fn_s3, fn_s4])

        except _Stop:
            pass
        S.add("sp", lambda e: e.nop(), extra_deps=stores + list(dbg_out.values()))
        build_program.stats = S.stats()
        S.emit()
    return nc


_CACHE = {}


def _host_consts():
    flags = []
    invs = []
    wins = (2, 4, 8, 16)
    for core in range(8):
        odd = core % 2
        flags.append(np.full((128, 1), float(odd), np.float32))
        iv = np.zeros((128, 64), np.float32)
        for g, w in enumerate(wins):
            for t in range(16):
                tg = t + (NT if odd else 0)
                iv[:, g * 16 + t] = 1.0 / min(tg + 1, w)
        invs.append(iv)
    return flags, invs


def kernel(**inputs):
    debug = tuple(inputs.pop("_debug", ()))
    x = np.ascontiguousarray(inputs["x"], dtype=np.float32)
    p = np.ascontiguousarray(inputs["p"], dtype=np.float32)[0]
    key = ("prog", debug)
    if key not in _CACHE:
        _CACHE[key] = build_program(debug)
    nc = _CACHE[key]
    w = {}
    for nm, shp in W_SPECS:
        a = np.asarray(inputs[nm], dtype=np.float32)
        w[nm] = np.ascontiguousarray(a.reshape(shp))
    flags, invs = _host_consts()
    zeros = np.zeros((NT, D), np.float32)
    in_maps = []
    for core in range(8):
        b, half = core // 2, core % 2
        m = dict(w)
        m["xm"] = np.ascontiguousarray(x[b, half * NT:(half + 1) * NT])
        m["xp"] = np.ascontiguousarray(x[b, 0:NT]) if half == 1 else zeros
        m["pm"] = np.ascontiguousarray(p[b, half * NT:(half + 1) * NT])
        m["flag"] = flags[core]
        m["invcnt"] = invs[core]
        in_maps.append(m)
    res = run_bass_kernel_spmd(nc, in_maps, core_ids=list(range(8)))
    outp = np.empty((4, 2048, D), np.float32)
    for core in range(8):
        b, half = core // 2, core % 2
        outp[b, half * NT:(half + 1) * NT] = res.results[core]["out"]
    if debug:
        kernel.last_debug = [{kk: vv for kk, vv in r.items() if kk.startswith("dbg_")} for r in res.results]
    return outp
```

```python
import numpy as np
import concourse.bass as bass
import concourse.mybir as mybir
from contextlib import ExitStack

F32 = mybir.dt.float32
BF16 = mybir.dt.bfloat16
I32 = mybir.dt.int32
AF = mybir.ActivationFunctionType
ALU = mybir.AluOpType
AX = mybir.AxisListType
_DSZ = {F32: 4, BF16: 2, mybir.dt.int32: 4, mybir.dt.uint32: 4}


def _region(ap):
    tname = type(ap.tensor).__name__
    if "DRam" in tname:
        return None
    key = "PS" if "PSum" in tname else ap.tensor.name
    sz = _DSZ[ap.dtype]
    dims = ap.ap
    pstep, pcnt = dims[0]
    off = int(ap.offset)
    if pstep > 0:
        p0 = off // pstep
        f0 = off % pstep
    else:
        p0, f0 = 0, off
    free = [(s, c) for (s, c) in dims[1:] if c > 1]
    if key == "PS":
        ext = 1
        for s_, c_ in free:
            ext += (c_ - 1) * abs(s_)
        lo = (f0 * sz) // 2048 * 2048
        hi = -(-((f0 + ext) * sz) // 2048) * 2048
        return key, 0, 128, [(lo, hi)]
    if not free:
        return key, p0, p0 + pcnt, [(f0 * sz, (f0 + 1) * sz)]
    ls, lc = free[-1]
    outer = free[:-1]
    n_outer = 1
    for s, c in outer:
        n_outer *= c
    run = (lc - 1) * abs(ls) + 1
    if n_outer <= 64:
        offs = [0]
        for s, c in outer:
            offs = [o + i * s for o in offs for i in range(c)]
        ivs = sorted((f0 + o) for o in offs)
        out = []
        for o in ivs:
            lo, hi = o * sz, (o + run) * sz
            if out and lo <= out[-1][1]:
                out[-1] = (out[-1][0], max(out[-1][1], hi))
            else:
                out.append((lo, hi))
        return key, p0, p0 + pcnt, out
    ext = run
    for s, c in outer:
        ext += (c - 1) * abs(s)
    return key, p0, p0 + pcnt, [(f0 * sz, (f0 + ext) * sz)]


def _iv_overlap(a, b):
    i = j = 0
    while i < len(a) and j < len(b):
        if a[i][1] <= b[j][0]:
            i += 1
        elif b[j][1] <= a[i][0]:
            j += 1
        else:
            return True
    return False


def _iv_covers(a, b):
    for lo, hi in b:
        ok = False
        for alo, ahi in a:
            if alo <= lo and hi <= ahi:
                ok = True
                break
        if not ok:
            return False
    return True


class Op:
    __slots__ = ("eng", "fn", "tl", "tpos", "waits", "need_inc", "semval", "done", "is_dma", "idx", "extra", "cond")


class Sched:
    ENGS = ("pe", "act", "dve", "pool", "sp")

    def __init__(self, nc):
        self.nc = nc
        self.ops = []
        self.by_eng = {e: [] for e in self.ENGS}
        self.eclock = {e: {} for e in self.ENGS}
        self.recs = {}
        self.tl_count = {}
        self.dma_group_total = {}
        self._cond = None
        self._cond_snap = None
        self.regs = {}
        self.dummy = {}

    def cond_begin(self, tag, regkey):
        if self._cond is None:
            self._cond = ()
            self._cond_snaps = []
        self._cond = self._cond + ((tag, regkey),)
        self._cond_snaps.append({e: dict(c) for e, c in self.eclock.items()})
        self._cond_snap = self._cond_snaps[0]

    def cond_end(self):
        self.eclock = self._cond_snaps.pop()
        self._cond = self._cond[:-1]
        if not self._cond:
            self._cond = None
            self._cond_snap = None

    def flagload(self, eng, key, flag_ap):
        def fn(e, eng=eng, key=key):
            if (eng, key) not in self.regs:
                self.regs[(eng, key)] = e.alloc_register("fl_%s_%s" % (eng, key))
            return e.reg_load(self.regs[(eng, key)], flag_ap)
        return self.add(eng, fn, reads=[flag_ap])

    def add(self, eng, fn, reads=(), writes=(), dma_key=None, extra_deps=()):
        op = Op()
        op.eng = eng
        op.fn = fn
        op.is_dma = dma_key is not None
        op.tl = ("dma", dma_key) if op.is_dma else eng
        op.tpos = self.tl_count.get(op.tl, 0) + 1
        self.tl_count[op.tl] = op.tpos
        op.need_inc = op.is_dma
        op.semval = None
        op.waits = []
        op.idx = len(self.ops)
        op.extra = None
        op.cond = self._cond if self._cond is not None else ()
        assert not (op.is_dma and op.cond)
        deps = list(extra_deps)
        accs = []
        for ap in reads:
            r = _region(ap)
            if r is not None:
                accs.append((r, False))
        for ap in writes:
            r = _region(ap)
            if r is not None:
                accs.append((r, True))
        for (key, p0, p1, ivs), is_w in accs:
            lst = self.recs.setdefault(key, [])
            for rec in lst:
                if not (rec[3] or is_w) and key != "PS":
                    continue
                if rec[1] <= p0 or p1 <= rec[0]:
                    continue
                if not _iv_overlap(rec[2], ivs):
                    continue
                d = rec[4]
                if d is op:
                    continue
                if (not op.is_dma) and (not d.is_dma) and d.eng == eng:
                    if eng == "pe":
                        continue
                    if not (rec[3] or is_w):
                        continue
                deps.append(d)
        clock = dict(self.eclock[eng])
        deps.sort(key=lambda d: -d.idx)
        for d in deps:
            if clock.get(d.tl, 0) >= d.tpos:
                continue
            op.waits.append(d)
            d.need_inc = True
            for k, v in d.done.items():
                if clock.get(k, 0) < v:
                    clock[k] = v
        self.eclock[eng] = clock
        if op.cond:
            op.done = dict(self._cond_snap[eng])
        else:
            op.done = dict(clock)
        op.done[op.tl] = op.tpos
        for (key, p0, p1, ivs), is_w in accs:
            lst = self.recs[key]
            if is_w:
                lst[:] = [r for r in lst if not (p0 <= r[0] and r[1] <= p1 and _iv_covers(ivs, r[2]))]
            else:
                lst[:] = [r for r in lst if not ((not r[3]) and r[4].tl == op.tl and r[0] == p0 and r[1] == p1 and r[2] == ivs)]
            lst.append([p0, p1, ivs, is_w, op])
        self.ops.append(op)
        self.by_eng[eng].append(op)
        return op

    def emit(self):
        nc = self.nc
        cnt = {}
        for op in self.ops:
            if op.is_dma:
                op.semval = 16 * op.tpos
            elif op.need_inc:
                cnt[op.tl] = cnt.get(op.tl, 0) + 1
                op.semval = cnt[op.tl]
        for k, v in cnt.items():
            assert v < 60000, (k, v)
        tls = sorted({op.tl for op in self.ops if op.need_inc}, key=str)
        with ExitStack() as es:
            sems = {}
            for i, tl in enumerate(tls):
                nm = "s_" + ("_".join(str(x) for x in tl) if isinstance(tl, tuple) else tl)
                sems[tl] = es.enter_context(nc.semaphore(nm))
            block = es.enter_context(nc.Block())

            def run(engname):
                def emit_op(eng, op):
                    for d in op.waits:
                        eng.wait_ge(sems[d.tl], d.semval)
                    ins = op.fn(eng)
                    if op.need_inc:
                        ins.then_inc(sems[op.tl], 16 if op.is_dma else 1)

                def emit_seq(eng, ops, depth):
                    i = 0
                    while i < len(ops):
                        op = ops[i]
                        if len(op.cond) <= depth:
                            emit_op(eng, op)
                            i += 1
                            continue
                        key = op.cond[depth]
                        j = i
                        while j < len(ops) and len(ops[j].cond) > depth and ops[j].cond[depth] == key:
                            j += 1
                        grp = ops[i:j]
                        ninc = sum(1 for o in grp if o.need_inc)
                        r = self.regs[(engname, key[1])]
                        with eng.If_ne(r, 0):
                            emit_seq(eng, grp, depth + 1)
                        if ninc > 0:
                            with eng.Else():
                                self.dummy[engname](eng).then_inc(sems[engname], ninc)
                        i = j

                def body(eng):
                    emit_seq(eng, self.by_eng[engname], 0)
                return body

            if self.by_eng["pe"]:
                block.tensor(run("pe"))
            if self.by_eng["act"]:
                block.scalar(run("act"))
            if self.by_eng["dve"]:
                block.vector(run("dve"))
            if self.by_eng["pool"]:
                block.gpsimd(run("pool"))
            if self.by_eng["sp"]:
                block.sync(run("sp"))

    def stats(self):
        out = {}
        for e in self.ENGS:
            ops = self.by_eng[e]
            out[e] = (len(ops), sum(len(o.waits) for o in ops), sum(1 for o in ops if o.need_inc))
        return out


def _isap(x):
    return hasattr(x, "ap") and hasattr(x, "tensor")


class K:
    def __init__(self, S):
        self.S = S
        self._dk = 0

    def mm(self, out, lhsT, rhs, start=True, stop=True):
        return self.S.add("pe", lambda e: e.matmul(out, lhsT, rhs, start=start, stop=stop),
                          reads=[lhsT, rhs], writes=[out])

    def tr(self, out, in_, ident):
        return self.S.add("pe", lambda e: e.transpose(out, in_, ident), reads=[in_, ident], writes=[out])

    def act(self, out, in_, func, bias=0.0, scale=1.0, accum_out=None):
        reads = [in_] + [x for x in (bias, scale) if _isap(x)]
        writes = [out] + ([accum_out] if accum_out is not None else [])
        if accum_out is not None:
            fn = lambda e: e.activation(out, in_, func, bias=bias, scale=scale, accum_out=accum_out)
        else:
            fn = lambda e: e.activation(out, in_, func, bias=bias, scale=scale)
        return self.S.add("act", fn, reads=reads, writes=writes)

    def tt(self, out, in0, in1, op, eng="dve"):
        return self.S.add(eng, lambda e: e.tensor_tensor(out, in0, in1, op), reads=[in0, in1], writes=[out])

    def ts(self, out, in0, s1, s2, op0, op1=None, eng="dve", accum_out=None):
        reads = [in0] + [x for x in (s1, s2) if _isap(x)]
        writes = [out] + ([accum_out] if accum_out is not None else [])
        if op1 is None:
            fn = lambda e: e.tensor_single_scalar(out, in0, s1, op0)
        elif accum_out is not None:
            fn = lambda e: e.tensor_scalar(out, in0, s1, s2, op0, op1, accum_out=accum_out)
        else:
            fn = lambda e: e.tensor_scalar(out, in0, s1, s2, op0, op1)
        return self.S.add(eng, fn, reads=reads, writes=writes)

    def stt(self, out, in0, scalar, in1, op0, op1, eng="dve"):
        reads = [in0, in1] + ([scalar] if _isap(scalar) else [])
        return self.S.add(eng, lambda e: e.scalar_tensor_tensor(out, in0, scalar, in1, op0, op1),
                          reads=reads, writes=[out])

    def scan(self, out, d0, d1, initial, op0=ALU.mult, op1=ALU.add):
        reads = [d0, d1] + ([initial] if _isap(initial) else [])
        return self.S.add("dve", lambda e: e.tensor_tensor_scan(out, d0, d1, initial, op0, op1),
                          reads=reads, writes=[out])

    def copy(self, out, in_, eng="dve"):
        return self.S.add(eng, lambda e: e.tensor_copy(out, in_), reads=[in_], writes=[out])

    def memset(self, out, val, eng="dve"):
        return self.S.add(eng, lambda e: e.memset(out, val), reads=[], writes=[out])

    def recip(self, out, in_):
        return self.S.add("dve", lambda e: e.reciprocal(out, in_), reads=[in_], writes=[out])

    def reduce(self, out, in_, op, axis=AX.X):
        return self.S.add("dve", lambda e: e.tensor_reduce(out, in_, axis, op), reads=[in_], writes=[out])

    def dma(self, out, in_, q="pool", key=None):
        if key is None:
            self._dk += 1
            key = "u%d" % self._dk
        return self.S.add(q, lambda e: e.dma_start(out=out, in_=in_), reads=[in_], writes=[out], dma_key=key)


from concourse.bass_utils import run_bass_kernel_spmd

KB = 1024
NT = 1024
D = 2048
KC = 16
EPS = 1e-6
A0, A1, RING, TT, CC = 0, 32 * KB, 96 * KB, 160 * KB, 196 * KB
ARENA_BYTES = 207 * KB
NSLOT = 8

W_SPECS = [
    ("norm1_g", [2048]), ("w_in", [2048, 7168]), ("conv_w", [4, 1024]), ("conv_b", [1024]),
    ("w_rg_a", [16, 64, 64]), ("b_rg_a", [1024]), ("w_rg_x", [16, 64, 64]), ("b_rg_x", [1024]),
    ("lru_lambda", [1024]), ("w_pool", [4, 256, 256]), ("pool_scale", [1024]),
    ("w_branch_a", [1024, 2048]), ("w_branch_b", [1024, 2048]), ("w_out", [2048, 2048]),
    ("norm2_g", [2048]), ("w_router_group", [2048, 4]), ("b_router_group", [4]),
    ("w_router_expert", [2048, 16]), ("b_router_expert", [16]),
    ("w_e_gate", [16, 2048, 512]), ("w_e_up", [16, 2048, 512]), ("w_e_down", [16, 512, 2048]),
    ("norm_ple_g", [2048]), ("w_ple_gate", [2048, 2048]), ("w_ple_proj", [256, 2048]),
    ("final_norm_g", [2048]),
]


class _Stop(Exception):
    pass


def pipeline(n, stages):
    for it in range(n + len(stages) - 1):
        for si in range(len(stages) - 1, -1, -1):
            t = it - si
            if 0 <= t < n:
                stages[si](t)


def build_program(debug=(), stop_after=None):
    nc = bass.Bass("TRN2", target_bir_lowering=False)
    dr = {}
    for nm, shp in [("xm", [NT, D]), ("xp", [NT, D]), ("pm", [NT, 256]), ("flag", [128, 1]), ("invcnt", [128, 64])] + W_SPECS:
        dr[nm] = nc.dram_tensor(nm, shp, F32, kind="ExternalInput").ap()
    out = nc.dram_tensor("out", [NT, D], F32, kind="ExternalOutput").ap()
    x1s = nc.dram_tensor("x1_scratch", [NT, D], F32, kind="Internal").ap()
    dbg_out = {}
    es = ExitStack()
    with es:
        A = es.enter_context(nc.sbuf_tensor("arena", [128, ARENA_BYTES // 2], BF16))
        PS = es.enter_context(nc.psum_tensor("ps", [128, 8, 512], F32))
        S = Sched(nc)
        k = K(S)

        def view(off, shape, dt=BF16):
            n = 1
            for s in shape[1:]:
                n *= s
            e0 = off // 2
            ne = n * (2 if dt in (F32, I32) else 1)
            assert off % 4 == 0 and off + ne * 2 <= ARENA_BYTES, (off, shape)
            v = A[:, e0:e0 + ne]
            if dt in (F32, I32):
                v = v.bitcast(dt)
            if len(shape) == 3:
                v = v.rearrange("p (a b) -> p a b", a=shape[1])
            elif len(shape) == 4:
                v = v.rearrange("p (a b c) -> p a b c", a=shape[1], b=shape[2])
            if shape[0] < 128:
                v = v[0:shape[0]]
            return v

        bank_ctr = [0]

        held = set()

        def nb():
            assert len(held) < 7, "all PSUM banks held"
            while True:
                b = bank_ctr[0] % 7
                bank_ctr[0] += 1
                if b not in held:
                    return b

        def psb(b):
            return PS[:, b, :].bitcast(BF16)

        slot_ctr = [0]

        ring_addrs = [RING + i * 8 * KB for i in range(NSLOT)]

        def wload(src, shape):
            s = slot_ctr[0] % len(ring_addrs)
            slot_ctr[0] += 1
            v = view(ring_addrs[s], shape, BF16)
            k.dma(v, src, q="pool", key="ring%d" % s)
            return v

        def dump(name, v):
            if name in debug:
                t = nc.dram_tensor("dbg_" + name, list(v.shape), v.dtype, kind="ExternalOutput").ap()
                dbg_out[name] = k.dma(t, v, q="sp")
            if stop_after == name:
                raise _Stop()

        stores = []
        try:
            ident_bf = view(CC + 0, [128, 128], BF16)
            ident_f = view(CC + 256, [128, 128], F32)
            chv = view(CC + 768, [128, 9, 8], F32)
            c1h = view(CC + 1056, [128, 8], F32)
            c1f = view(CC + 1088, [128, 8], F32)
            stg = view(CC + 1120, [72, 128], F32)
            wr = view(CC + 1632, [128, 16, 20], F32)
            rb = view(CC + 2912, [128, 20], F32)
            invc = view(CC + 2992, [128, 64], F32)
            flg = view(CC + 3248, [128, 1], F32)
            bh = view(CC + 3264, [128, 2, 8], F32)
            ss = view(CC + 3328, [128, 16], F32)
            tmpv = view(CC + 3392, [128, 16], F32)
            rstd = view(CC + 3456, [128, 16], F32)
            stt_ = view(CC + 3520, [128, 8], F32)
            tmp16 = view(CC + 3552, [128, 16], F32)
            lg = view(CC + 3616, [128, 8, 20], F32)
            comb = view(CC + 4256, [128, 8, 16], F32)
            rsc = [view(CC + 4768 + i * 128, [128, 8, 4], F32) for i in range(12)]
            rs1 = [view(CC + 6304 + i * 32, [128, 8], F32) for i in range(8)]
            prod = view(CC + 6560, [128, 8, 4, 4], F32)

            k.memset(ident_f, 0.0)
            S.add("pool", lambda e: e.affine_select(out=ident_f, in_=ident_f, pattern=[[-1, 128]],
                                                    compare_op=ALU.not_equal, fill=1.0, base=0, channel_multiplier=1),
                  reads=[ident_f], writes=[ident_f])
            k.copy(ident_bf, ident_f)
            vecs = [dr["conv_w"][0], dr["conv_w"][1], dr["conv_w"][2], dr["conv_w"][3], dr["conv_b"],
                    dr["b_rg_a"], dr["b_rg_x"], dr["lru_lambda"], dr["pool_scale"]]
            for vi, vsrc in enumerate(vecs):
                k.dma(stg[vi * 8:(vi + 1) * 8, :], vsrc.rearrange("(c p) -> c p", p=128), q="sp")
            b0 = nb()
            k.tr(PS[:, b0, 0:72], stg, ident_f[0:72, 0:72])
            k.copy(chv.rearrange("p v c -> p (v c)"), PS[:, b0, 0:72])
            k.dma(flg, dr["flag"], q="sp")
            k.dma(invc, dr["invcnt"], q="sp")
            k.act(c1f, chv[:, 7, :], AF.Exp, scale=-1.0)
            k.act(c1f, c1f, AF.Ln, bias=1.0)
            k.ts(c1h, c1f, -4.0, None, ALU.mult)
            k.ts(c1f, c1f, -8.0, None, ALU.mult)
            k.ts(bh.rearrange("p a b -> p (a b)"), chv[:, 5:7, :].rearrange("p a b -> p (a b)"), 0.5, None, ALU.mult)
            k.dma(wr[:, :, 0:4], dr["w_router_group"].rearrange("(kc p) n -> p kc n", p=128), q="sp")
            k.dma(wr[:, :, 4:20], dr["w_router_expert"].rearrange("(kc p) n -> p kc n", p=128), q="sp")
            k.dma(rb[:, 0:4], dr["b_router_group"].partition_broadcast(128), q="sp")
            k.dma(rb[:, 4:20], dr["b_router_expert"].partition_broadcast(128), q="sp")

            hTp = view(A0, [128, 16, NT], BF16)
            uT = hTp
            htT = hTp
            hpT = hTp
            hT = view(A1, [128, 16, NT], BF16)
            y_a = view(A1 + 32 * KB, [128, 8, NT], BF16)
            y_b = view(A1 + 48 * KB, [128, 8, NT], BF16)
            x1v = view(A1, [128, 8, D], F32)

            def rms_stats(src, col):
                return col

            xld = [view(A1 + 32 * KB + i * 8 * KB, [128, D], F32) for i in range(4)]
            gbc = view(TT + 16 * KB, [128, D], F32)
            xnbs = [view(TT + 24 * KB, [128, D], BF16), view(TT + 28 * KB, [128, D], BF16)]
            k.dma(gbc, dr["norm1_g"].partition_broadcast(128), q="sp")
            evc = [0]
            sqjunk = view(TT + 32 * KB, [128, D], BF16)
            n1banks = {}

            def n1_s0(t):
                src = dr["xp"] if t < 8 else dr["xm"]
                r0 = (t % 8) * 128
                xl = xld[t % 4]
                k.dma(xl, src[r0:r0 + 128, :], q="sp", key="xld%d" % (t % 4))
                k.act(sqjunk, xl, AF.Square, accum_out=ss[:, t:t + 1])

            def n1_s1(t):
                k.ts(tmpv[:, t:t + 1], ss[:, t:t + 1], 1.0 / D, EPS, ALU.mult, ALU.add)

            def n1_s2(t):
                k.act(tmpv[:, t:t + 1], tmpv[:, t:t + 1], AF.Sqrt)

            def n1_s3(t):
                k.recip(rstd[:, t:t + 1], tmpv[:, t:t + 1])
                k.stt(xnbs[t % 2], xld[t % 4], rstd[:, t:t + 1], gbc, ALU.mult, ALU.mult)

            def n1_s4(t):
                xnb = xnbs[t % 2]
                bl = []
                for h in range(2):
                    b = nb()
                    held.add(b)
                    bl.append(b)
                    pb = psb(b)
                    for j in range(8):
                        kc = h * 8 + j
                        k.tr(pb[:, j * 128:(j + 1) * 128], xnb[:, kc * 128:(kc + 1) * 128], ident_bf)
                n1banks[t] = bl

            def n1_s5(t):
                r0 = (t % 8) * 128
                dstT = hTp if t < 8 else hT
                for h, b in enumerate(n1banks[t]):
                    dst = dstT[:, h * 8:(h + 1) * 8, r0:r0 + 128]
                    srcp = psb(b).rearrange("p (a b) -> p a b", a=8)
                    if h == 0:
                        k.act(dst, srcp, AF.Identity)
                    else:
                        k.copy(dst, srcp)
                    held.discard(b)

            pipeline(16, [n1_s0, n1_s1, n1_s2, n1_s3, n1_s4, n1_s5])
            dump("hT", hT)
            dump("hTp", hTp)

            YB0 = A1 + 48 * KB
            Wbd = view(YB0 + 12 * KB, [128, 8, 2, 128], BF16)
            k.memset(Wbd.rearrange("p a b c -> p (a b c)"), 0.0)
            for gi, wnm in enumerate(["w_rg_a", "w_rg_x"]):
                wsrc = dr[wnm].rearrange("(c h) i j -> h i c j", h=2)
                for h in range(2):
                    k.dma(Wbd[h * 64:(h + 1) * 64, :, gi, h * 64:(h + 1) * 64], wsrc[h], q="pool")
            RS7 = ring_addrs.pop(7)
            xr2 = [view(TT + i * 4112, [128, 1027], F32) for i in range(2)]
            xc3 = [view(TT + 8224, [128, NT], F32), view(TT + 12320, [128, NT], F32), view(RS7, [128, NT], F32)]
            xcb2 = [view(RS7 + 4096 + i * 2048, [128, NT], BF16) for i in range(2)]
            Ab2 = [view(TT + 16416 + i * 4096, [128, NT], F32) for i in range(2)]
            Gb2 = [view(TT + 24608 + i * 4096, [128, NT], F32) for i in range(2)]
            Mb2 = [view(YB0 + i * 4096, [128, NT], F32) for i in range(2)]
            Yb = view(YB0 + 8 * KB, [128, NT], F32)
            halo_sv = view(CC + 11248, [128, 3], F32)
            w_in_v = dr["w_in"].rearrange("(kc p) c -> p kc c", p=128)
            chains = [(c, hf) for c in range(8) for hf in range(2)]
            wxs, wgs_ = {}, {}
            zbanks, ybanks = {}, {}

            def rn0(t):
                c, hf = chains[t]
                cc = c % 2
                if cc == 0 and hf == 0:
                    wxs[c // 2] = wload(w_in_v[:, :, c * 128:c * 128 + 256], [128, 16, 256])
                    wgs_[c // 2] = wload(w_in_v[:, :, 1024 + c * 128:1024 + c * 128 + 256], [128, 16, 256])
                wx = wxs[c // 2]
                hsrc = hTp if hf == 0 else hT
                bl = []
                for blk in range(2):
                    b = nb()
                    held.add(b)
                    bl.append(b)
                    for kc in range(KC):
                        k.mm(PS[:, b, :], wx[:, kc, cc * 128:(cc + 1) * 128], hsrc[:, kc, blk * 512:(blk + 1) * 512],
                             start=(kc == 0), stop=(kc == KC - 1))
                zbanks[t] = bl

            def rn1(t):
                xr = xr2[t % 2]
                for blk, b in enumerate(zbanks[t]):
                    k.act(xr[:, 3 + blk * 512:3 + (blk + 1) * 512], PS[:, b, :], AF.Identity)
                    held.discard(b)

            def rn2(t):
                c, hf = chains[t]
                cc = c % 2
                xr, xc, xcb = xr2[t % 2], xc3[t % 3], xcb2[t % 2]
                if hf == 0:
                    k.memset(xr[:, 0:3], 0.0)
                else:
                    k.copy(xr[:, 0:3], halo_sv)
                k.ts(xc, xr[:, 3:3 + NT], chv[:, 3, c:c + 1], chv[:, 4, c:c + 1], ALU.mult, ALU.add)
                for kk in range(3):
                    k.stt(xc, xr[:, kk:kk + NT], chv[:, kk, c:c + 1], xc, ALU.mult, ALU.add)
                if hf == 0:
                    k.copy(halo_sv, xr[:, NT:NT + 3])
                k.copy(xcb, xc)
                if hf == 1:
                    wg = wgs_[c // 2]
                    bl = []
                    for blk in range(2):
                        b = nb()
                        held.add(b)
                        bl.append(b)
                        for kc in range(KC):
                            k.mm(PS[:, b, :], wg[:, kc, cc * 128:(cc + 1) * 128], hT[:, kc, blk * 512:(blk + 1) * 512],
                                 start=(kc == 0), stop=(kc == KC - 1))
                    ybanks[t] = bl

            def rn3(t):
                c, hf = chains[t]
                xcb, Ab, Gb, Mb = xcb2[t % 2], Ab2[t % 2], Gb2[t % 2], Mb2[t % 2]
                for gi, buf in enumerate([Ab, Gb]):
                    for blk in range(2):
                        b = nb()
                        k.mm(PS[:, b, :], Wbd[:, c, gi, :], xcb[:, blk * 512:(blk + 1) * 512])
                        k.act(buf[:, blk * 512:(blk + 1) * 512], PS[:, b, :], AF.Tanh,
                              bias=bh[:, gi, c:c + 1], scale=0.5)
                k.act(Mb, Ab, AF.Exp, bias=c1f[:, c:c + 1], scale=c1f[:, c:c + 1])
                k.act(Ab, Ab, AF.Exp, bias=c1h[:, c:c + 1], scale=c1h[:, c:c + 1])
                k.act(Mb, Mb, AF.Sqrt, bias=1.0, scale=-1.0)
                if hf == 1:
                    for blk, b in enumerate(ybanks[t]):
                        k.act(Yb[:, blk * 512:(blk + 1) * 512], PS[:, b, :], AF.Gelu_apprx_tanh)
                        held.discard(b)

            def rn4(t):
                c, hf = chains[t]
                xc, Ab, Gb, Mb = xc3[t % 3], Ab2[t % 2], Gb2[t % 2], Mb2[t % 2]
                k.stt(Gb, Gb, 1.0, Mb, ALU.add, ALU.mult)
                k.stt(Gb, Gb, 0.5, xc, ALU.mult, ALU.mult)
                if hf == 0:
                    k.scan(Mb, Ab, Gb, 0.0)
                    k.tt(stt_[:, c:c + 1], Mb[:, NT - 1:NT], flg, ALU.mult)
                else:
                    k.scan(Mb, Ab, Gb, stt_[:, c:c + 1])
                    k.tt(y_a[:, c, :], Yb, Mb, ALU.mult)

            pipeline(16, [rn0, rn1, rn2, rn3, rn4])
            ring_addrs.insert(7, RS7)
            dump("y_a", y_a)

            L = 16 + NT
            P0s = [view(TT + i * 4160, [128, L], F32) for i in range(2)]
            P1 = view(TT + 8320, [128, L], F32)
            P2 = view(TT + 12480, [128, L], F32)
            dbfs = [view(TT + 16640 + i * 4096, [128, 2, NT], BF16) for i in range(2)]
            wpl = view(TT + 24832, [128, 4, 2, 256], BF16)
            k.dma(wpl, dr["w_pool"].rearrange("g (kc p) d -> p g kc d", p=128), q="pool")
            wps, pbanks = {}, {}

            def pz(pc):
                cc = pc % 2
                if cc == 0:
                    wps[pc // 2] = wload(w_in_v[:, :, 2048 + pc * 128:2048 + pc * 128 + 256], [128, 16, 256])
                wp = wps[pc // 2]
                bl = []
                b = nb()
                held.add(b)
                bl.append(b)
                for kc in range(KC):
                    k.mm(PS[:, b, 0:16], wp[:, kc, cc * 128:(cc + 1) * 128], hTp[:, kc, NT - 16:NT],
                         start=(kc == 0), stop=(kc == KC - 1))
                for blk in range(2):
                    b = nb()
                    held.add(b)
                    bl.append(b)
                    for kc in range(KC):
                        k.mm(PS[:, b, :], wp[:, kc, cc * 128:(cc + 1) * 128], hT[:, kc, blk * 512:(blk + 1) * 512],
                             start=(kc == 0), stop=(kc == KC - 1))
                pbanks[pc] = bl

            def pev(pc):
                P0 = P0s[pc % 2]
                bl = pbanks[pc]
                k.act(P0[:, 0:16], PS[:, bl[0], 0:16], AF.Identity)
                held.discard(bl[0])
                for blk in range(2):
                    k.act(P0[:, 16 + blk * 512:16 + (blk + 1) * 512], PS[:, bl[1 + blk], :], AF.Identity)
                    held.discard(bl[1 + blk])

            def pd(pc):
                cc = pc % 2
                g = pc // 2
                w = 2 << g
                P0 = P0s[pc % 2]
                dbf = dbfs[g % 2]
                cur, oth = P0, [P1, P2]
                sh = 1
                for st in range(g + 1):
                    dst = oth[st % 2]
                    lo = 2 * sh - 1
                    k.tt(dst[:, lo:L], cur[:, lo:L], cur[:, lo - sh:L - sh], ALU.add)
                    cur = dst
                    sh *= 2
                k.stt(dbf[:, cc, :], cur[:, 16:L], 1.0 / w, P0[:, 16:L], ALU.mult, ALU.subtract)
                k.tt(tmp16, cur[:, 16:32], invc[:, g * 16:(g + 1) * 16], ALU.mult)
                k.tt(dbf[:, cc, 0:16], tmp16, P0[:, 16:32], ALU.subtract)

            def pm(pc):
                if pc % 2 == 0:
                    return
                g = pc // 2
                dbf = dbfs[g % 2]
                for oc in range(2):
                    for blk in range(2):
                        b = nb()
                        for kc2 in range(2):
                            k.mm(PS[:, b, :], wpl[:, g, kc2, oc * 128:(oc + 1) * 128], dbf[:, kc2, blk * 512:(blk + 1) * 512],
                                 start=(kc2 == 0), stop=(kc2 == 1))
                        k.act(y_b[:, g * 2 + oc, blk * 512:(blk + 1) * 512], PS[:, b, :], AF.Identity,
                              scale=chv[:, 8, g * 2 + oc:g * 2 + oc + 1])

            pipeline(8, [pz, pev, pd, pm])
            dump("y_b", y_b)

            sA = [view(TT + i * 2 * KB, [128, 512], F32) for i in range(2)]
            sB = [view(TT + 4 * KB + i * 2 * KB, [128, 512], F32) for i in range(2)]
            t1 = [view(TT + 8 * KB + i * 2 * KB, [128, 512], F32) for i in range(2)]
            wa_v = dr["w_branch_a"].rearrange("(kc p) c -> p kc c", p=128)
            wb_v = dr["w_branch_b"].rearrange("(kc p) c -> p kc c", p=128)
            ga = gb = wA = wB = None
            it = 0
            for oc in range(16):
                if oc % 4 == 0:
                    wA = wload(wa_v[:, :, oc * 128:oc * 128 + 512], [128, 8, 512])
                    wB = wload(wb_v[:, :, oc * 128:oc * 128 + 512], [128, 8, 512])
                if oc % 2 == 0:
                    ga = wload(w_in_v[:, :, 3072 + oc * 128:3072 + oc * 128 + 256], [128, 16, 256])
                    gb = wload(w_in_v[:, :, 5120 + oc * 128:5120 + oc * 128 + 256], [128, 16, 256])
                c2 = oc % 2
                c4 = oc % 4
                for blk in range(2):
                    bs = slice(blk * 512, (blk + 1) * 512)
                    i2 = it % 2
                    it += 1
                    b1 = nb()
                    for kc in range(KC):
                        k.mm(PS[:, b1, :], ga[:, kc, c2 * 128:(c2 + 1) * 128], hT[:, kc, bs], start=(kc == 0), stop=(kc == KC - 1))
                    k.act(sA[i2], PS[:, b1, :], AF.Sigmoid)
                    b2 = nb()
                    for kc in range(8):
                        k.mm(PS[:, b2, :], wA[:, kc, c4 * 128:(c4 + 1) * 128], y_a[:, kc, bs], start=(kc == 0), stop=(kc == 7))
                    k.tt(sA[i2], sA[i2], PS[:, b2, :], ALU.mult)
                    b3 = nb()
                    for kc in range(KC):
                        k.mm(PS[:, b3, :], gb[:, kc, c2 * 128:(c2 + 1) * 128], hT[:, kc, bs], start=(kc == 0), stop=(kc == KC - 1))
                    k.act(sB[i2], PS[:, b3, :], AF.Sigmoid)
                    b4 = nb()
                    for kc in range(8):
                        k.mm(PS[:, b4, :], wB[:, kc, c4 * 128:(c4 + 1) * 128], y_b[:, kc, bs], start=(kc == 0), stop=(kc == 7))
                    k.tt(sB[i2], sB[i2], PS[:, b4, :], ALU.mult)
                    k.tt(uT[:, oc, bs], sA[i2], sB[i2], ALU.add)
            dump("uT", uT)

            xrl1 = view(TT + 28 * KB, [128, D], F32)
            wo_v = dr["w_out"].rearrange("(kc p) c -> p kc c", p=128)
            wo_all = [[wload(wo_v[:, h * 8:(h + 1) * 8, db * 512:(db + 1) * 512], [128, 8, 512]) for h in range(2)]
                      for db in range(4)]
            gbc2 = view(TT + 0, [128, D], F32)
            xn32s = [view(TT + 8 * KB, [128, D], F32)] * 2
            ht32T = view(TT + 16 * KB, [128, 16, 128], F32)
            k.dma(gbc2, dr["norm2_g"].partition_broadcast(128), q="sp")

            sqjunk2 = view(TT + 24 * KB, [128, D], BF16)
            rtbanks, rtb2 = {}, {}

            def rt_s0(i):
                k.act(sqjunk2, x1v[:, i, :], AF.Square, accum_out=ss[:, i:i + 1])

            def rt_s1(i):
                k.ts(tmpv[:, i:i + 1], ss[:, i:i + 1], 1.0 / D, EPS, ALU.mult, ALU.add)

            def rt_s2(i):
                k.act(tmpv[:, i:i + 1], tmpv[:, i:i + 1], AF.Sqrt)

            def rt_s3(i):
                k.recip(rstd[:, i:i + 1], tmpv[:, i:i + 1])
                k.stt(xn32s[i % 2], x1v[:, i, :], rstd[:, i:i + 1], gbc2, ALU.mult, ALU.mult)

            def rt_s4(i):
                xn32 = xn32s[i % 2]
                bl = []
                for q in range(4):
                    b = nb()
                    held.add(b)
                    bl.append(b)
                    for j in range(4):
                        kc = q * 4 + j
                        k.tr(PS[:, b, j * 128:(j + 1) * 128], xn32[:, kc * 128:(kc + 1) * 128], ident_f)
                rtbanks[i] = bl

            def rt_s5(i):
                for q, b in enumerate(rtbanks[i]):
                    srcp = PS[:, b, :].rearrange("p (a b) -> p a b", a=4)
                    if q % 2 == 0:
                        k.act(ht32T[:, q * 4:(q + 1) * 4, :], srcp, AF.Identity)
                    else:
                        k.copy(ht32T[:, q * 4:(q + 1) * 4, :], srcp)
                    held.discard(b)
                b = nb()
                held.add(b)
                rtb2[i] = b
                for kc in range(KC):
                    k.mm(PS[:, b, 0:20], ht32T[:, kc, :], wr[:, kc, :], start=(kc == 0), stop=(kc == KC - 1))

            def rt_s6(i):
                b = rtb2[i]
                k.tt(lg[:, i, :], PS[:, b, 0:20], rb, ALU.add)
                held.discard(b)


            def wout_tile(i):
                k.dma(xrl1, dr["xm"][i * 128:(i + 1) * 128, :], q="sp", key="xrl1")
                for db in range(4):
                    ds_ = slice(db * 512, (db + 1) * 512)
                    b = nb()
                    for kc in range(KC):
                        k.mm(PS[:, b, :], uT[:, kc, i * 128:(i + 1) * 128], wo_all[db][kc // 8][:, kc % 8, :],
                             start=(kc == 0), stop=(kc == KC - 1))
                    k.tt(x1v[:, i, ds_], xrl1[:, ds_], PS[:, b, :], ALU.add)

            pipeline(8, [wout_tile, rt_s0, rt_s1, rt_s2, rt_s3, rt_s4, rt_s5, rt_s6])
            x1s_w = []
            for i in range(8):
                x1s_w.append(k.dma(x1s[i * 128:(i + 1) * 128, :], x1v[:, i, :], q="sp"))
            dump("x1", x1v)

            dmyA = view(CC + 7072, [128, 1], F32)
            dmyD = view(CC + 7088, [128, 1], F32)
            k.memset(dmyA, 0.0)
            k.memset(dmyD, 0.0)
            S.dummy["pe"] = lambda e: e.matmul(PS[:, 7, 0:2], ident_bf, ident_bf[:, 0:2], start=True, stop=True)
            S.dummy["act"] = lambda e: e.activation(dmyA, dmyA, AF.Identity)
            S.dummy["dve"] = lambda e: e.memset(dmyD, 0.0)
            Lf = view(CC + 7104, [128, 128], F32)
            Ltri = view(CC + 7616, [128, 128], BF16)
            ones_bf = view(CC + 7872, [128, 128], BF16)
            siota = view(CC + 8128, [128, 8], F32)
            jv = view(CC + 8160, [128, 8], F32)
            oh_bf = view(CC + 8192, [128, 32], BF16)
            tot_s = view(CC + 8256, [128, 8, 4], F32)
            wit_s = view(CC + 8384, [128, 8, 4], F32)
            tpre = view(CC + 8512, [128, 8, 4], F32)
            ngv = view(CC + 8640, [128, 4], F32)
            offv = view(CC + 8656, [128, 4], F32)
            endv = view(CC + 8672, [128, 4], F32)
            posv = view(CC + 8688, [128, 8], F32)
            fl_a = view(CC + 8720, [128, 4, 8], F32)
            fl_b = view(CC + 8848, [128, 4, 8], F32)
            fl_i = view(CC + 8976, [128, 4, 8], I32)
            cmb_hl = view(CC + 9104, [128, 8, 32], BF16)
            cmb_t = view(CC + 9616, [128, 8, 16], F32)
            comb_s = view(CC + 10128, [128, 8, 16], F32)
            iint = view(CC + 10640, [128, 16], I32)
            umask = view(CC + 10832, [128, 4, 8], F32)
            flU = view(CC + 10960, [128, 4], F32)
            flU_i = view(CC + 10976, [128, 4], I32)
            LIKELY = [(0, 3), (1, 5), (3, 7), (5, 8)]
            c32 = view(CC + 10704, [128, 32], F32)
            k.memset(Lf, 1.0)
            S.add("pool", lambda e: e.affine_select(out=Lf, in_=Lf, pattern=[[1, 128]], compare_op=ALU.is_gt,
                                                    fill=0.0, base=0, channel_multiplier=-1), reads=[Lf], writes=[Lf])
            k.copy(Ltri, Lf)
            k.memset(Lf, 1.0)
            k.memset(ones_bf, 1.0)
            S.add("pool", lambda e: e.iota(iint[:, 0:8], pattern=[[128, 8]], base=0, channel_multiplier=1), writes=[iint[:, 0:8]])
            S.add("pool", lambda e: e.iota(iint[:, 8:16], pattern=[[128, 8]], base=0, channel_multiplier=0), writes=[iint[:, 8:16]])
            k.copy(siota, iint[:, 0:8])
            k.copy(jv, iint[:, 8:16])

            dump("lg", lg)
            lgG = lg[:, :, 0:4]
            lgE4 = lg[:, :, 4:20].rearrange("p t (g j) -> p t j g", g=4)
            mx, sg_, gw, m1, m2, se, gw2 = rs1[0], rs1[1], rs1[2], rs1[3], rs1[4], rs1[5], rs1[6]
            oh, eg, lsel, msk, le2, sel, ee, wt4 = rsc[0], rsc[1], rsc[2], rsc[3], rsc[4], rsc[5], rsc[6], rsc[7]
            basev, posg = rsc[8], rsc[9]

            def bc3(v):
                return v.unsqueeze(2).to_broadcast([128, 8, 4])
            k.reduce(mx, lgG, ALU.max)
            k.tt(oh, lgG, bc3(mx), ALU.is_equal)
            k.tt(eg, lgG, bc3(mx), ALU.subtract)
            k.act(eg, eg, AF.Exp)
            k.reduce(sg_, eg, ALU.add)
            k.recip(gw, sg_)
            k.tt(prod, lgE4, oh.unsqueeze(2).to_broadcast([128, 8, 4, 4]), ALU.mult)
            k.reduce(lsel, prod, ALU.add)
            k.reduce(m1, lsel, ALU.max)
            k.tt(msk, lsel, bc3(m1), ALU.is_equal)
            k.stt(le2, msk, -1e30, lsel, ALU.mult, ALU.add)
            k.reduce(m2, le2, ALU.max)
            k.tt(sel, lsel, bc3(m2), ALU.is_ge)
            k.tt(ee, lsel, bc3(m1), ALU.subtract)
            k.act(ee, ee, AF.Exp)
            k.tt(ee, ee, sel, ALU.mult)
            k.reduce(se, ee, ALU.add)
            k.recip(gw2, se)
            k.tt(gw2, gw2, gw, ALU.mult)
            k.tt(wt4, ee, bc3(gw2), ALU.mult)
            comb4 = comb.rearrange("p t (g j) -> p t g j", g=4)
            k.tt(comb4, oh.unsqueeze(3).to_broadcast([128, 8, 4, 4]), wt4.unsqueeze(2).to_broadcast([128, 8, 4, 4]), ALU.mult)
            dump("comb", comb)

            k.copy(oh_bf, oh.rearrange("p t g -> p (t g)"))
            b = nb()
            k.mm(PS[:, b, 0:32], ones_bf, oh_bf)
            k.copy(tot_s.rearrange("p t g -> p (t g)"), PS[:, b, 0:32])
            b = nb()
            k.mm(PS[:, b, 0:32], Ltri, oh_bf)
            k.copy(wit_s.rearrange("p t g -> p (t g)"), PS[:, b, 0:32])
            k.memset(tpre[:, 0, :], 0.0)
            for i in range(1, 8):
                k.tt(tpre[:, i, :], tpre[:, i - 1, :], tot_s[:, i - 1, :], ALU.add)
            k.tt(ngv, tpre[:, 7, :], tot_s[:, 7, :], ALU.add)
            k.memset(offv[:, 0:1], 0.0)
            for g in range(1, 4):
                k.tt(offv[:, g:g + 1], offv[:, g - 1:g], ngv[:, g - 1:g], ALU.add)
            k.tt(endv, offv, ngv, ALU.add)
            k.tt(basev, tpre, offv.unsqueeze(1).to_broadcast([128, 8, 4]), ALU.add)
            k.tt(basev, basev, wit_s, ALU.add)
            k.tt(posg, basev, oh, ALU.mult)
            k.reduce(posv, posg, ALU.add)
            offb = offv.unsqueeze(2).to_broadcast([128, 4, 8])
            endb = endv.unsqueeze(2).to_broadcast([128, 4, 8])
            jvb = jv.unsqueeze(1).to_broadcast([128, 4, 8])
            k.tt(fl_a, offb, jvb, ALU.subtract)
            k.ts(fl_a, fl_a, 128.0, None, ALU.is_lt)
            k.tt(fl_b, endb, jvb, ALU.subtract)
            k.ts(fl_b, fl_b, 0.0, None, ALU.is_gt)
            k.tt(fl_a, fl_a, fl_b, ALU.mult)
            k.copy(fl_i, fl_a)
            k.memset(umask.rearrange("p g j -> p (g j)"), 1.0)
            for g in range(4):
                lo_, hi_ = LIKELY[g]
                k.memset(umask[:, g, lo_:hi_], 0.0)
            k.tt(umask, umask, fl_a, ALU.mult)
            k.reduce(flU, umask, ALU.max)
            k.copy(flU_i, flU)
            dump("posv", posv)
            dump("fl_a", fl_a)

            iota_i = view(A0, [128, NT], I32)
            iota_s = view(A0 + 4 * KB, [128, NT], F32)
            Pm = view(TT + 0, [128, 8, NT], BF16)
            S.add("pool", lambda e: e.iota(iota_i, pattern=[[1, NT]], base=0, channel_multiplier=0), writes=[iota_i])
            k.copy(iota_s, iota_i)
            for i in range(8):
                k.ts(Pm[:, i, :], iota_s, posv[:, i:i + 1], None, ALU.is_equal)
            k.copy(cmb_hl[:, :, 0:16], comb)
            k.tt(cmb_t, comb, cmb_hl[:, :, 0:16], ALU.subtract)
            k.copy(cmb_hl[:, :, 16:32], cmb_t)
            for j in range(8):
                b = nb()
                for i in range(8):
                    k.mm(PS[:, b, 0:32], Pm[:, i, j * 128:(j + 1) * 128], cmb_hl[:, i, :], start=(i == 0), stop=(i == 7))
                k.copy(c32, PS[:, b, 0:32])
                k.tt(comb_s[:, j, :], c32[:, 0:16], c32[:, 16:32], ALU.add)
            dump("comb_s", comb_s)

            ht_tok = view(TT + 16 * KB, [128, 8, 1024], BF16)
            gbh = view(TT + 32 * KB, [128, 1024], F32)
            htS = hTp
            ev = 0
            for hh in range(2):
                fsl = slice(hh * 1024, (hh + 1) * 1024)
                k.dma(gbh, dr["norm2_g"][fsl].partition_broadcast(128), q="sp", key="gbh")
                for i in range(8):
                    k.stt(ht_tok[:, i, :], x1v[:, i, fsl], rstd[:, i:i + 1], gbh, ALU.mult, ALU.mult)
                for kc8 in range(8):
                    kc = hh * 8 + kc8
                    for sb in range(2):
                        b = nb()
                        for i in range(8):
                            k.mm(PS[:, b, :], ht_tok[:, i, kc8 * 128:(kc8 + 1) * 128], Pm[:, i, sb * 512:(sb + 1) * 512],
                                 start=(i == 0), stop=(i == 7))
                        dst = htS[:, kc, sb * 512:(sb + 1) * 512]
                        if ev % 2 == 0:
                            k.act(dst, PS[:, b, :], AF.Identity)
                        else:
                            k.copy(dst, PS[:, b, :])
                        ev += 1
            dump("htS", htS)

            ysum = x1v
            for j in range(8):
                k.memset(ysum[:, j, :], 0.0)

            slb = [view(TT + i * 2 * KB, [128, 512], F32) for i in range(2)]
            atok = [view(TT + 4 * KB + i * KB, [128, 512], BF16) for i in range(2)]
            aT_all = [view(TT + 6 * KB, [128, 8, 4, 128], BF16)] * 2
            ring_addrs.extend([TT + 16 * KB, TT + 24 * KB])
            bi = 0
            for g in range(4):
                for en in ("pe", "act", "dve"):
                    for j in range(8):
                        S.flagload(en, "j%d" % j, fl_i[0:1, g, j:j + 1])
                    S.flagload(en, "U", flU_i[0:1, g:g + 1])
                lo_, hi_ = LIKELY[g]
                jorder = list(range(lo_, hi_)) + [j for j in range(8) if not (lo_ <= j < hi_)]
                for e4 in range(4):
                    e = g * 4 + e4
                    wgv = dr["w_e_gate"][e].rearrange("(kc p) f -> p kc f", p=128)
                    wuv = dr["w_e_up"][e].rearrange("(kc p) f -> p kc f", p=128)
                    wdv = dr["w_e_down"][e].rearrange("(fc p) d -> p fc d", p=128)
                    wgs = [wload(wgv[:, h * 8:(h + 1) * 8, :], [128, 8, 512]) for h in range(2)]
                    wus = [wload(wuv[:, h * 8:(h + 1) * 8, :], [128, 8, 512]) for h in range(2)]
                    wds = [wload(wdv[:, :, h * 1024:(h + 1) * 1024], [128, 4, 1024]) for h in range(2)]
                    aTe = aT_all[e % 2]
                    in_outer = False
                    for j in jorder:
                        unlikely = not (lo_ <= j < hi_)
                        if unlikely and not in_outer:
                            S.cond_begin(("Ua", g, e4), "U")
                            in_outer = True
                        S.cond_begin((g, e4, j, "a"), "j%d" % j)
                        js = slice(j * 128, (j + 1) * 128)
                        bg = nb()
                        for kc in range(KC):
                            k.mm(PS[:, bg, :], htS[:, kc, js], wgs[kc // 8][:, kc % 8, :], start=(kc == 0), stop=(kc == KC - 1))
                        bu = nb()
                        for kc in range(KC):
                            k.mm(PS[:, bu, :], htS[:, kc, js], wus[kc // 8][:, kc % 8, :], start=(kc == 0), stop=(kc == KC - 1))
                        sl = slb[bi % 2]
                        at = atok[bi % 2]
                        bi += 1
                        k.act(sl, PS[:, bg, :], AF.Silu)
                        k.tt(at, sl, PS[:, bu, :], ALU.mult)
                        bt = nb()
                        pbt = psb(bt)
                        for fc in range(4):
                            k.tr(pbt[:, fc * 128:(fc + 1) * 128], at[:, fc * 128:(fc + 1) * 128], ident_bf)
                        k.act(aTe[:, j, :, :], pbt[:, 0:512].rearrange("p (a b) -> p a b", a=4), AF.Identity)
                        S.cond_end()
                    if in_outer:
                        S.cond_end()
                    in_outer = False
                    for j in jorder:
                        unlikely = not (lo_ <= j < hi_)
                        if unlikely and not in_outer:
                            S.cond_begin(("Ub", g, e4), "U")
                            in_outer = True
                        S.cond_begin((g, e4, j, "b"), "j%d" % j)
                        for db in range(4):
                            b = nb()
                            for fc in range(4):
                                k.mm(PS[:, b, :], aTe[:, j, fc, :], wds[db // 2][:, fc, (db % 2) * 512:(db % 2 + 1) * 512],
                                     start=(fc == 0), stop=(fc == 3))
                            ys = ysum[:, j, db * 512:(db + 1) * 512]
                            k.stt(ys, PS[:, b, :], comb_s[:, j, e:e + 1], ys, ALU.mult, ALU.add)
                        S.cond_end()
                    if in_outer:
                        S.cond_end()
            del ring_addrs[NSLOT:]
            dump("ysum", ysum)

            posrow = view(TT + 8 * KB, [128, NT], F32)
            dg = view(TT + 12 * KB, [128, 128], F32)
            PT = view(TT + 16 * KB, [128, 8, NT], BF16)
            yh = view(A0, [128, 8, 1024], BF16)
            yl = view(A0 + 16 * KB, [128, 8, 1024], BF16)
            xn32 = xn32s[0]
            ytmp = view(TT + 32 * KB, [128, 1024], F32)
            for hb in range(2):
                b = nb()
                for i4 in range(4):
                    i = hb * 4 + i4
                    k.ts(dg, ident_f, posv[:, i:i + 1], None, ALU.mult)
                    k.mm(PS[:, b, i4 * 128:(i4 + 1) * 128], Lf, dg)
                k.copy(posrow[:, hb * 512:(hb + 1) * 512], PS[:, b, :])
            for j in range(8):
                k.ts(PT[:, j, :], posrow, siota[:, j:j + 1], None, ALU.is_equal)
            cv = 0
            yhs = [yh, yl]
            for dh in range(2):
                dsl = slice(dh * 1024, (dh + 1) * 1024)
                yh = yhs[dh]
                for j in range(8):
                    if j % 2 == 0:
                        k.act(yh[:, j, :], ysum[:, j, dsl], AF.Copy)
                    else:
                        k.copy(yh[:, j, :], ysum[:, j, dsl])
                for i in range(8):
                    dst = x1v[:, i, dsl]
                    src = x1s[i * 128:(i + 1) * 128, dsl]
                    S.add("sp", (lambda e, dst=dst, src=src: e.dma_start(out=dst, in_=src)), reads=[], writes=[dst],
                          dma_key="x1r%d_%d" % (dh, i), extra_deps=[x1s_w[i]])
                for i in range(8):
                    for d2 in range(2):
                        b = nb()
                        n = 0
                        for j in range(8):
                            k.mm(PS[:, b, :], PT[:, j, i * 128:(i + 1) * 128], yh[:, j, d2 * 512:(d2 + 1) * 512],
                                 start=(j == 0), stop=(j == 7))
                        dcol = slice(dh * 1024 + d2 * 512, dh * 1024 + (d2 + 1) * 512)
                        k.tt(x1v[:, i, dcol], x1v[:, i, dcol], PS[:, b, :], ALU.add)
            dump("x2", x1v)

            gbc3 = view(TT + 0, [128, D], F32)
            xnb3s = [view(TT + 8 * KB + i * 4 * KB, [128, D], BF16) for i in range(2)]
            pT = view(TT + 16 * KB, [128, 2, NT], BF16)
            pl = [view(TT + 20 * KB + i * KB, [128, 256], F32) for i in range(2)]
            pbf = view(TT + 22 * KB, [128, 256], BF16)
            sg3 = [view(TT + 23 * KB + i * 2 * KB, [128, 512], F32) for i in range(2)]
            sqjunk3 = view(TT + 27 * KB, [128, D], BF16)
            ost = [view(TT + 8 * KB + i * 8 * KB, [128, D], F32) for i in range(2)]
            k.dma(gbc3, dr["norm_ple_g"].partition_broadcast(128), q="sp")
            for i in range(8):
                k.dma(pl[i % 2], dr["pm"][i * 128:(i + 1) * 128, :], q="sp", key="pl%d" % (i % 2))
                k.copy(pbf, pl[i % 2])
                b = nb()
                pb = psb(b)
                for j in range(2):
                    k.tr(pb[:, j * 128:(j + 1) * 128], pbf[:, j * 128:(j + 1) * 128], ident_bf)
                k.copy(pT[:, :, i * 128:(i + 1) * 128], pb[:, 0:256].rearrange("p (a b) -> p a b", a=2))
            plbanks = {}

            def pl_s0(i):
                k.act(sqjunk3, x1v[:, i, :], AF.Square, accum_out=ss[:, i:i + 1])

            def pl_s1(i):
                k.ts(tmpv[:, i:i + 1], ss[:, i:i + 1], 1.0 / D, EPS, ALU.mult, ALU.add)

            def pl_s2(i):
                k.act(tmpv[:, i:i + 1], tmpv[:, i:i + 1], AF.Sqrt)

            def pl_s3(i):
                k.recip(rstd[:, i:i + 1], tmpv[:, i:i + 1])
                k.stt(xnb3s[i % 2], x1v[:, i, :], rstd[:, i:i + 1], gbc3, ALU.mult, ALU.mult)

            def pl_s4(i):
                xnb3 = xnb3s[i % 2]
                bl = []
                for h in range(2):
                    b = nb()
                    held.add(b)
                    bl.append(b)
                    pb = psb(b)
                    for j in range(8):
                        kc = h * 8 + j
                        k.tr(pb[:, j * 128:(j + 1) * 128], xnb3[:, kc * 128:(kc + 1) * 128], ident_bf)
                plbanks[i] = bl

            def pl_s5(i):
                for h, b in enumerate(plbanks[i]):
                    dst = hpT[:, h * 8:(h + 1) * 8, i * 128:(i + 1) * 128]
                    srcp = psb(b).rearrange("p (a b) -> p a b", a=8)
                    if h == 0:
                        k.act(dst, srcp, AF.Identity)
                    else:
                        k.copy(dst, srcp)
                    held.discard(b)

            pipeline(8, [pl_s0, pl_s1, pl_s2, pl_s3, pl_s4, pl_s5])
            wpp_v = dr["w_ple_proj"].rearrange("(kc p) d -> p kc d", p=128)
            wpg_v = dr["w_ple_gate"].rearrange("(kc p) c -> p kc c", p=128)
            it = 0
            for db in range(4):
                ds_ = slice(db * 512, (db + 1) * 512)
                wpg = [wload(wpg_v[:, h * 8:(h + 1) * 8, ds_], [128, 8, 512]) for h in range(2)]
                wpp = wload(wpp_v[:, :, ds_], [128, 2, 512])
                for i in range(8):
                    ts_ = slice(i * 128, (i + 1) * 128)
                    b1 = nb()
                    for kc in range(KC):
                        k.mm(PS[:, b1, :], hpT[:, kc, ts_], wpg[kc // 8][:, kc % 8, :], start=(kc == 0), stop=(kc == KC - 1))
                    b2 = nb()
                    for kc2 in range(2):
                        k.mm(PS[:, b2, :], pT[:, kc2, ts_], wpp[:, kc2, :], start=(kc2 == 0), stop=(kc2 == 1))
                    sgt = sg3[it % 2]
                    it += 1
                    k.act(sgt, PS[:, b1, :], AF.Sigmoid)
                    k.tt(sgt, sgt, PS[:, b2, :], ALU.mult)
                    k.tt(x1v[:, i, ds_], x1v[:, i, ds_], sgt, ALU.add)
            k.dma(gbc3, dr["final_norm_g"].partition_broadcast(128), q="sp")
            def fn_s0(i):
                k.act(sqjunk3, x1v[:, i, :], AF.Square, accum_out=ss[:, 8 + i:9 + i])

            def fn_s1(i):
                k.ts(tmpv[:, 8 + i:9 + i], ss[:, 8 + i:9 + i], 1.0 / D, EPS, ALU.mult, ALU.add)

            def fn_s2(i):
                k.act(tmpv[:, 8 + i:9 + i], tmpv[:, 8 + i:9 + i], AF.Sqrt)

            def fn_s3(i):
                k.recip(rstd[:, 8 + i:9 + i], tmpv[:, 8 + i:9 + i])
                k.stt(ost[i % 2], x1v[:, i, :], rstd[:, 8 + i:9 + i], gbc3, ALU.mult, ALU.mult)

            def fn_s4(i):
                stores.append(k.dma(out[i * 128:(i + 1) * 128, :], ost[i % 2], q="sp", key="ost%d" % (i % 2)))

            pipeline(8, [fn_s0, fn_s1, fn_s2, fn_s3, fn_s4])

        except _Stop:
            pass
        S.add("sp", lambda e: e.nop(), extra_deps=stores + list(dbg_out.values()))
        build_program.stats = S.stats()
        S.emit()
    return nc


_CACHE = {}


def _host_consts():
    flags = []
    invs = []
    wins = (2, 4, 8, 16)
    for core in range(8):
        odd = core % 2
        flags.append(np.full((128, 1), float(odd), np.float32))
        iv = np.zeros((128, 64), np.float32)
        for g, w in enumerate(wins):
            for t in range(16):
                tg = t + (NT if odd else 0)
                iv[:, g * 16 + t] = 1.0 / min(tg + 1, w)
        invs.append(iv)
    return flags, invs


def kernel(**inputs):
    debug = tuple(inputs.pop("_debug", ()))
    x = np.ascontiguousarray(inputs["x"], dtype=np.float32)
    p = np.ascontiguousarray(inputs["p"], dtype=np.float32)[0]
    key = ("prog", debug)
    if key not in _CACHE:
        _CACHE[key] = build_program(debug)
    nc = _CACHE[key]
    w = {}
    for nm, shp in W_SPECS:
        a = np.asarray(inputs[nm], dtype=np.float32)
        w[nm] = np.ascontiguousarray(a.reshape(shp))
    flags, invs = _host_consts()
    zeros = np.zeros((NT, D), np.float32)
    in_maps = []
    for core in range(8):
        b, half = core // 2, core % 2
        m = dict(w)
        m["xm"] = np.ascontiguousarray(x[b, half * NT:(half + 1) * NT])
        m["xp"] = np.ascontiguousarray(x[b, 0:NT]) if half == 1 else zeros
        m["pm"] = np.ascontiguousarray(p[b, half * NT:(half + 1) * NT])
        m["flag"] = flags[core]
        m["invcnt"] = invs[core]
        in_maps.append(m)
    res = run_bass_kernel_spmd(nc, in_maps, core_ids=list(range(8)))
    outp = np.empty((4, 2048, D), np.float32)
    for core in range(8):
        b, half = core // 2, core % 2
        outp[b, half * NT:(half + 1) * NT] = res.results[core]["out"]
    if debug:
        kernel.last_debug = [{kk: vv for kk, vv in r.items() if kk.startswith("dbg_")} for r in res.results]
    return outp
```
